# Optimizing a Trainium2 kernel written in Bass

```python
import math
import jax, jax.numpy as jnp
from jax import lax
import numpy as np

D_MODEL = 1024
BATCH = 4
SEQ = 8192
DEPTH = 1

MIX_WIDTH = D_MODEL
POOL_WIDTH = MIX_WIDTH // 2
POOL_WINDOWS = (2, 4, 8, 16)
N_POOL_GROUPS = len(POOL_WINDOWS)
POOL_GROUP_WIDTH = POOL_WIDTH // N_POOL_GROUPS
ATTN_WIDTH = MIX_WIDTH - POOL_WIDTH
SB_HEAD_DIM = 64
SB_HEADS = ATTN_WIDTH // SB_HEAD_DIM
Q_BLOCK = 128
IN_WIDTH = POOL_WIDTH + 3 * ATTN_WIDTH
MEM_LEN = 256
MEM_HEADS = 4
MEM_HEAD_DIM = D_MODEL // MEM_HEADS
N_GROUPS = 4
EXPERTS_PER_GROUP = 4
N_EXPERTS = N_GROUPS * EXPERTS_PER_GROUP
TOP_K_EXPERT = 2
EXPERT_FF = D_MODEL // 2
RMS_EPS = 1e-6

kernel_name = "hybrid_pool_stickbreak_hmoe_layer"


def rms_norm(x, gain):
    xf = x.astype(jnp.float32)
    y = xf * lax.rsqrt(jnp.mean(xf * xf, axis=-1, keepdims=True) + RMS_EPS)
    return (y * gain.astype(jnp.float32)).astype(x.dtype)


def causal_multiscale_pool(u, w_pool, pool_scale):
    S = u.shape[1]
    t = jnp.arange(S)
    groups = jnp.split(u, N_POOL_GROUPS, axis=-1)
    outs = []
    for g, (ug, w) in enumerate(zip(groups, POOL_WINDOWS)):
        uf = ug.astype(jnp.float32)
        prefix = jnp.pad(jnp.cumsum(uf, axis=1), ((0, 0), (1, 0), (0, 0)))
        upper = prefix[:, 1:]
        lower = jnp.pad(prefix, ((0, 0), (w - 1, 0), (0, 0)))[:, :S]
        count = jnp.minimum(t + 1, w).astype(jnp.float32)[None, :, None]
        pooled = (upper - lower) / count - uf
        outs.append(jnp.einsum('bsc,cd->bsd', pooled.astype(u.dtype), w_pool[g]))
    return jnp.concatenate(outs, axis=-1) * pool_scale


def stick_breaking_attention(q, k, v):
    B, H, S, dh = q.shape
    n_blocks = S // Q_BLOCK
    scale = 1.0 / math.sqrt(dh)
    q_blocks = jnp.moveaxis(q.reshape(B, H, n_blocks, Q_BLOCK, dh), 2, 0)
    s_idx = jnp.arange(S)

    def one_block(args):
        qb, blk = args
        z = jnp.einsum('bhqd,bhkd->bhqk', qb, k).astype(jnp.float32) * scale
        t_idx = blk * Q_BLOCK + jnp.arange(Q_BLOCK)
        causal = s_idx[None, :] < t_idx[:, None]
        log_not = jnp.where(causal, jax.nn.log_sigmoid(-z), 0.0)
        suffix = lax.cumsum(log_not, axis=3, reverse=True) - log_not
        weights = jnp.where(causal, jnp.exp(jax.nn.log_sigmoid(z) + suffix), 0.0)
        return jnp.einsum('bhqk,bhkd->bhqd', weights.astype(v.dtype), v)

    out = lax.map(one_block, (q_blocks, jnp.arange(n_blocks)))
    return jnp.moveaxis(out, 0, 2).reshape(B, H, S, dh)


def memory_cross_attention(xn, memn, w_q, w_k, w_v, w_o):
    B, S, D = xn.shape
    q = jnp.einsum('bsd,de->bse', xn, w_q).reshape(B, S, MEM_HEADS, MEM_HEAD_DIM)
    k = jnp.einsum('bmd,de->bme', memn, w_k).reshape(B, -1, MEM_HEADS, MEM_HEAD_DIM)
    v = jnp.einsum('bmd,de->bme', memn, w_v).reshape(B, -1, MEM_HEADS, MEM_HEAD_DIM)
    s = jnp.einsum('bshd,bmhd->bhsm', q, k).astype(jnp.float32) / math.sqrt(MEM_HEAD_DIM)
    p = jax.nn.softmax(s, axis=-1).astype(v.dtype)
    o = jnp.einsum('bhsm,bmhd->bshd', p, v).reshape(B, S, D)
    return jnp.einsum('bse,ed->bsd', o, w_o)


def hierarchical_moe(x, w_group, w_expert, w_gate, w_up, w_down):
    B, S, D = x.shape
    xf = x.reshape(-1, D)
    g_prob = jax.nn.softmax(jnp.einsum('nd,dg->ng', xf, w_group).astype(jnp.float32), axis=-1)
    g_gate, g_idx = lax.top_k(g_prob, 1)
    e_logits = jnp.einsum('nd,de->ne', xf, w_expert).astype(jnp.float32)
    e_logits = e_logits.reshape(-1, N_GROUPS, EXPERTS_PER_GROUP)
    e_logits = jnp.take_along_axis(e_logits, g_idx[:, :, None], axis=1)[:, 0]
    e_prob = jax.nn.softmax(e_logits, axis=-1)
    e_w, e_idx = lax.top_k(e_prob, TOP_K_EXPERT)
    e_w = e_w / jnp.sum(e_w, axis=-1, keepdims=True)
    weights = g_gate * e_w
    expert_ids = g_idx * EXPERTS_PER_GROUP + e_idx
    combine = jnp.einsum('nk,nke->ne', weights,
                         jax.nn.one_hot(expert_ids, N_EXPERTS, dtype=jnp.float32))
    y = jnp.zeros(xf.shape, jnp.float32)
    for e in range(N_EXPERTS):
        h = jax.nn.silu(xf @ w_gate[e]) * (xf @ w_up[e])
        y = y + combine[:, e:e + 1] * (h @ w_down[e]).astype(jnp.float32)
    return y.astype(x.dtype).reshape(B, S, D)


def setup_inputs(seed: int = 0) -> dict:
    key = jax.random.key(seed)
    ks = jax.random.split(key, 20)
    f32 = jnp.float32

    def nrm(k, shape, fan_in):
        return jax.random.normal(k, shape, f32) * (fan_in ** -0.5)

    def gain(k, shape):
        return 1.0 + 0.02 * jax.random.normal(k, shape, f32)

    L = DEPTH
    return {
        "x": jax.random.normal(ks[0], (BATCH, SEQ, D_MODEL), f32),
        "mem": jax.random.normal(ks[1], (BATCH, MEM_LEN, D_MODEL), f32),
        "norm_mix": gain(ks[2], (L, D_MODEL)),
        "w_in": nrm(ks[3], (L, D_MODEL, IN_WIDTH), D_MODEL),
        "w_pool": nrm(ks[4], (L, N_POOL_GROUPS, POOL_GROUP_WIDTH, POOL_GROUP_WIDTH), POOL_GROUP_WIDTH),
        "pool_scale": gain(ks[5], (L, POOL_WIDTH)),
        "w_out": nrm(ks[6], (L, MIX_WIDTH, D_MODEL), MIX_WIDTH),
        "norm_cross": gain(ks[7], (L, D_MODEL)),
        "norm_mem": gain(ks[8], (L, D_MODEL)),
        "w_q_mem": nrm(ks[9], (L, D_MODEL, D_MODEL), D_MODEL),
        "w_k_mem": nrm(ks[10], (L, D_MODEL, D_MODEL), D_MODEL),
        "w_v_mem": nrm(ks[11], (L, D_MODEL, D_MODEL), D_MODEL),
        "w_o_mem": nrm(ks[12], (L, D_MODEL, D_MODEL), D_MODEL),
        "norm_ffn": gain(ks[13], (L, D_MODEL)),
        "w_group": nrm(ks[14], (L, D_MODEL, N_GROUPS), D_MODEL),
        "w_expert": nrm(ks[15], (L, D_MODEL, N_EXPERTS), D_MODEL),
        "w_gate": nrm(ks[16], (L, N_EXPERTS, D_MODEL, EXPERT_FF), D_MODEL),
        "w_up": nrm(ks[17], (L, N_EXPERTS, D_MODEL, EXPERT_FF), D_MODEL),
        "w_down": nrm(ks[18], (L, N_EXPERTS, EXPERT_FF, D_MODEL), EXPERT_FF),
        "norm_final": gain(ks[19], (D_MODEL,)),
    }


def reference(x, mem, norm_mix, w_in, w_pool, pool_scale, w_out, norm_cross, norm_mem,
              w_q_mem, w_k_mem, w_v_mem, w_o_mem, norm_ffn, w_group, w_expert,
              w_gate, w_up, w_down, norm_final):
    B, S, D = x.shape
    h = x
    for layer in range(DEPTH):
        xn = rms_norm(h, norm_mix[layer])
        proj = jnp.einsum('bsd,de->bse', xn, w_in[layer])
        u_pool = proj[..., :POOL_WIDTH]
        q, k, v = jnp.split(proj[..., POOL_WIDTH:], 3, axis=-1)
        to_heads = lambda t: jnp.transpose(t.reshape(B, S, SB_HEADS, SB_HEAD_DIM), (0, 2, 1, 3))
        pool_out = causal_multiscale_pool(u_pool, w_pool[layer], pool_scale[layer])
        attn_out = stick_breaking_attention(to_heads(q), to_heads(k), to_heads(v))
        attn_out = jnp.transpose(attn_out, (0, 2, 1, 3)).reshape(B, S, ATTN_WIDTH)
        mixed = jnp.concatenate([pool_out, attn_out], axis=-1)
        h = h + jnp.einsum('bse,ed->bsd', mixed, w_out[layer])
        h = h + memory_cross_attention(rms_norm(h, norm_cross[layer]), rms_norm(mem, norm_mem[layer]),
                                       w_q_mem[layer], w_k_mem[layer], w_v_mem[layer], w_o_mem[layer])
        h = h + hierarchical_moe(rms_norm(h, norm_ffn[layer]), w_group[layer], w_expert[layer],
                                 w_gate[layer], w_up[layer], w_down[layer])
    return rms_norm(h, norm_final)
```

```python
import numpy as np
from contextlib import ExitStack
import concourse.bass as bass
import concourse.mybir as mybir
from concourse.bass_utils import run_bass_kernel_spmd

F32 = mybir.dt.float32
BF16 = mybir.dt.bfloat16
AF = mybir.ActivationFunctionType
ALU = mybir.AluOpType
AX = mybir.AxisListType

D = 1024
S = 8192
NG = 16
OG = 8
NE = 16
FF = 512
EPS = 1e-6


class Tk:
    __slots__ = ("w", "r")

    def __init__(self):
        self.w = []
        self.r = {}


class Sched:
    def __init__(self, nc, ndma=16):
        self.nc = nc
        self.act = {}
        for name, eng in (("pe", nc.tensor), ("act", nc.scalar), ("dve", nc.vector),
                          ("pool", nc.gpsimd), ("sp", nc.sync)):
            self.act[name] = dict(eng=eng, sem=nc.alloc_semaphore("s_" + name), cnt=0, seen={}, key=name)
        self.dsem = {}
        self.dval = {}
        self.dnext = {}
        for q in ("sp", "pool"):
            for i in range(ndma):
                self.dsem[(q, i)] = nc.alloc_semaphore("d_%s%d" % (q, i))
                self.dval[(q, i)] = 0
            self.dnext[q] = 0
        self.ndma = ndma

    def _sem(self, key):
        return self.act[key]["sem"] if key in self.act else self.dsem[key]

    def _wait(self, a, deps):
        need = {}
        for k, v in deps:
            if a["seen"].get(k, 0) >= v:
                continue
            need[k] = max(need.get(k, 0), v)
        for k, v in need.items():
            a["eng"].wait_ge(self._sem(k), v)
            a["seen"][k] = v

    def _deps(self, a, reads, writes, acc=False):
        d = []
        me = a["key"]
        for b in reads:
            for t in b.w:
                if not (me == "pe" and t[0] == "pe"):
                    d.append(t)
        for b in writes:
            if not acc:
                for t in b.w:
                    if not (me == "pe" and t[0] == "pe"):
                        d.append(t)
            for k, v in b.r.items():
                if k != me:
                    d.append((k, v))
        return d

    def _commit(self, tok, reads, writes, acc):
        for b in reads:
            b.r[tok[0]] = tok[1]
        for b in writes:
            if acc:
                b.w = [t for t in b.w if t[0] != tok[0]] + [tok]
            else:
                b.w = [tok]
                b.r = {}

    def op(self, actor, fn, reads=(), writes=()):
        a = self.act[actor]
        self._wait(a, self._deps(a, reads, writes))
        inst = fn(a["eng"])
        a["cnt"] += 1
        inst.then_inc(a["sem"], 1)
        self._commit((a["key"], a["cnt"]), reads, writes, False)

    def dma(self, actor, out, in_, reads=(), writes=(), acc=False):
        a = self.act[actor]
        self._wait(a, self._deps(a, reads, writes, acc))
        i = self.dnext[actor]
        self.dnext[actor] = (i + 1) % self.ndma
        key = (actor, i)
        if self.dval[key] > 0:
            self._wait(a, [(key, self.dval[key])])
        inst = a["eng"].dma_start(out=out, in_=in_)
        self.dval[key] += 16
        inst.then_inc(self.dsem[key], 16)
        self._commit((key, self.dval[key]), reads, writes, acc)

    def barrier(self):
        toks = [(k, a["cnt"]) for k, a in self.act.items() if a["cnt"] > 0]
        toks += [(k, v) for k, v in self.dval.items() if v > 0]
        for a in self.act.values():
            self._wait(a, [t for t in toks if t[0] != a["key"]])


def build(dbg=False, stop_after=99):
    nc = bass.Bass("TRN2", target_bir_lowering=False)

    def din(name, shape, dt=F32):
        return nc.dram_tensor(name, list(shape), dt, kind="ExternalInput").ap()

    def dscr(name, shape, dt):
        return nc.dram_tensor(name, list(shape), dt, kind=("ExternalOutput" if dbg else "Internal")).ap()

    xf = din("xf", [S, D])
    xo = din("xo", [OG, 528, D])
    mem = din("mem", [256, D])
    w_in = din("w_in", [D, 2048])
    w_pool = din("w_pool", [4, 128, 128])
    pscale = din("pscale", [128, 4])
    w_out = din("w_out", [D, D])
    g_mix = din("g_mix", [D]); g_cross = din("g_cross", [D]); g_mem = din("g_mem", [D])
    g_ffn = din("g_ffn", [D]); g_fin = din("g_fin", [D])
    wq = din("wq", [D, D]); wk = din("wk", [D, D]); wv = din("wv", [D, D]); wo = din("wo", [D, D])
    w_ge = din("w_ge", [D, 20])
    w_gate = din("w_gate", [NE, D, FF]); w_up = din("w_up", [NE, D, FF]); w_down = din("w_down", [NE, FF, D])
    ident = din("ident", [128, 128]); ui_d = din("ui", [128, 128]); ls_d = din("ls", [128, 128])
    masks_d = din("masks", [8, 128, 512])
    invcnt_d = din("invcnt", [128, 2048])
    y = nc.dram_tensor("y", [OG * 512, D], F32, kind="ExternalOutput").ap()

    qT_d = dscr("qT_d", [OG, 128, 2048], BF16)
    poT_d = dscr("poT_d", [OG, 128, 2048], BF16)
    aT_d = dscr("aT_d", [OG, 64, 4096], BF16)
    h1_d = dscr("h1_d", [OG * 512, D], F32)
    h2_d = dscr("h2_d", [OG * 512, D], F32)
    wg16 = nc.dram_tensor("wg16", [NE, D, FF], BF16, kind="Internal").ap()
    wu16 = nc.dram_tensor("wu16", [NE, D, FF], BF16, kind="Internal").ap()
    wd16 = nc.dram_tensor("wd16", [NE, FF, D], BF16, kind="Internal").ap()
    t_wg16 = [Tk() for _ in range(NE)]; t_wu16 = [Tk() for _ in range(NE)]; t_wd16 = [Tk() for _ in range(NE)]
    t_q = [Tk() for _ in range(OG)]; t_po = [Tk() for _ in range(OG)]; t_a = [Tk() for _ in range(OG)]
    t_h1 = [Tk() for _ in range(OG * 4)]; t_h2 = [Tk() for _ in range(OG * 4)]

    Sc = Sched(nc)
    op = Sc.op
    dma = Sc.dma

    root = ExitStack()
    with root:
        def sb(es, name, shape, dt):
            return es.enter_context(nc.sbuf_tensor(name, list(shape), dt))

        PS = [root.enter_context(nc.psum_tensor("ps%d" % i, [128, 512], F32)) for i in range(8)]
        t_PS = [Tk() for _ in range(8)]

        idb = sb(root, "idb", [128, 128], BF16); t_c = Tk()
        uib = sb(root, "uib", [128, 128], BF16)
        lsb = sb(root, "lsb", [128, 128], BF16)
        epsb = sb(root, "epsb", [128, 1], F32)
        junk = sb(root, "junk", [128, 1024], BF16)
        stats = [sb(root, "stat%d" % i, [128, 4], F32) for i in range(4)]
        t_stats = [Tk() for _ in range(4)]
        rot = {"st": 0, "x": 0, "xn": 0, "tp": 0, "mm": 0}
        t_eps = Tk()
        dma("pool", idb[:, :], ident[:, :], writes=[t_c])
        dma("pool", uib[:, :], ui_d[:, :], writes=[t_c], acc=True)
        dma("pool", lsb[:, :], ls_d[:, :], writes=[t_c], acc=True)
        op("dve", lambda e: e.memset(epsb[:, :], EPS), writes=[t_eps])

        def load_gain(tile, t, vec):
            dma("sp", tile[:, :], vec.partition_broadcast(128), writes=[t])

        def load_w3(tile, t, src, nchunk, c0, c1, p=128):
            w = c1 - c0
            for dc in range(nchunk):
                dma("pool", tile[0:p, dc * w:(dc + 1) * w], src[dc * p:(dc + 1) * p, c0:c1], writes=[t], acc=(dc > 0))

        def norm_block(xin, t_x, gain, t_g, xout, t_out, P=128):
            k = rot["st"]; rot["st"] = (k + 1) % 4
            st, tst = stats[k], t_stats[k]
            op("act", lambda e: e.activation(out=junk[0:P, :], in_=xin, func=AF.Square, accum_out=st[0:P, 0:1]),
               reads=[t_x], writes=[tst])
            op("act", lambda e: e.activation(out=st[0:P, 1:2], in_=st[0:P, 0:1], func=AF.Ln, scale=1.0 / D,
                                             bias=epsb[0:P, 0:1]), reads=[tst, t_eps], writes=[tst])
            op("act", lambda e: e.activation(out=st[0:P, 2:3], in_=st[0:P, 1:2], func=AF.Exp, scale=-0.5),
               reads=[tst], writes=[tst])
            op("dve", lambda e: e.scalar_tensor_tensor(out=xout, in0=xin, scalar=st[0:P, 2:3], in1=gain[0:P, :],
                                                       op0=ALU.mult, op1=ALU.mult),
               reads=[t_x, tst, t_g], writes=[t_out])

        def transpose_block(xn, t_xn, dst3, t_dst, P=128):
            k = rot["tp"]; rot["tp"] = (k + 1) % 2
            bank, tb = PS[k], t_PS[k]
            psb = bank[:, :].bitcast(BF16)
            for dc in range(8):
                op("pe", lambda e, dc=dc: e.transpose(out=psb[:, dc * P:(dc + 1) * P], in_=xn[0:P, dc * 128:(dc + 1) * 128],
                                                      identity=idb[0:P, 0:P]), reads=[t_xn, t_c], writes=[tb])
            op("act", lambda e: e.activation(out=dst3, in_=psb[:, 0:8 * P].rearrange("p (c t) -> p c t", c=8), func=AF.Copy),
               reads=[tb], writes=[t_dst])

        def norm_transpose_gen(blocks, xnb, t_xnb):
            prev = None
            for bk in list(blocks) + [None]:
                if bk is not None:
                    if bk.get("load"):
                        bk["load"]()
                    kn = rot["xn"]; rot["xn"] = (kn + 1) % 2
                    P = bk.get("P", 128)
                    norm_block(bk["xin"], bk["t_x"], bk["gain"], bk["t_g"], xnb[kn][0:P, :], t_xnb[kn], P=P)
                    bk["kn"] = kn
                if prev is not None:
                    transpose_block(xnb[prev["kn"]], t_xnb[prev["kn"]], prev["dst3"], prev["t_dst"], P=prev.get("P", 128))
                    if prev.get("post"):
                        prev["post"]()
                prev = bk
                yield

        def norm_transpose_seq(blocks, xnb, t_xnb):
            for _ in norm_transpose_gen(blocks, xnb, t_xnb):
                pass

        def drain(gen):
            if gen is not None:
                for _ in gen:
                    pass

        def mm_bank():
            k = rot["mm"]; rot["mm"] = (k + 1) % 6
            return PS[2 + k], t_PS[2 + k]

        def v3(tile, c):
            return tile[:, :].rearrange("p (c t) -> p c t", c=c)

        with ExitStack() as es:
            winPQ = sb(es, "winPQ", [128, 8 * 1024], BF16); t_winPQ = Tk()
            wpb = sb(es, "wpb", [128, 4 * 128], BF16); t_wpb = Tk()
            psc = sb(es, "psc", [128, 4], F32); t_psc = Tk()
            gmix = sb(es, "gmix", [128, D], F32); t_gmix = Tk()
            invc = sb(es, "invc", [128, 2048], F32); t_invc = Tk()
            xb = [sb(es, "xb%d" % i, [128, D], F32) for i in range(3)]; t_xb = [Tk() for _ in range(3)]
            xnb = [sb(es, "xnb%d" % i, [128, D], BF16) for i in range(2)]; t_xnb = [Tk() for _ in range(2)]
            xnT = [sb(es, "xnT%d" % i, [128, 8 * 528], BF16) for i in range(2)]; t_xnT = [Tk() for _ in range(2)]
            QTst = [sb(es, "QTst%d" % i, [128, 2048], BF16) for i in range(2)]; t_QTst = [Tk() for _ in range(2)]
            uT = sb(es, "uT", [128, 4 * 528], F32); t_uT = Tk()
            tA = [sb(es, "tA%d" % i, [128, 528], F32) for i in range(4)]; t_tA = [Tk() for _ in range(4)]
            tB = [sb(es, "tB%d" % i, [128, 528], F32) for i in range(4)]; t_tB = [Tk() for _ in range(4)]
            pooled = sb(es, "pooled", [128, 2048], BF16); t_pooled = [Tk() for _ in range(4)]
            poSt = [sb(es, "poSt%d" % i, [128, 2048], BF16) for i in range(2)]; t_poSt = [Tk() for _ in range(2)]

            load_gain(gmix, t_gmix, g_mix)
            load_w3(winPQ, t_winPQ, w_in, 8, 0, 1024)
            for pg in range(4):
                dma("pool", wpb[:, pg * 128:(pg + 1) * 128], w_pool[pg, :, :], writes=[t_wpb], acc=(pg > 0))
            dma("sp", psc[:, :], pscale[:, :], writes=[t_psc])
            dma("sp", invc[:, :], invcnt_d[:, :], writes=[t_invc])

            def prep_1b(i):
                xT, t_xT = xnT[i % 2], t_xnT[i % 2]
                xT3 = v3(xT, 8)
                blocks = []
                for blk in range(-1, 4):
                    P = 16 if blk < 0 else 128
                    r0 = 0 if blk < 0 else 16 + blk * 128
                    kx = rot["x"]; rot["x"] = (kx + 1) % 3
                    blocks.append(dict(
                        load=(lambda kx=kx, P=P, r0=r0, i=i: dma("sp", xb[kx][0:P, :], xo[i, r0:r0 + P, :], writes=[t_xb[kx]])),
                        xin=xb[kx][0:P, :], t_x=t_xb[kx], gain=gmix, t_g=t_gmix, P=P,
                        dst3=xT3[:, :, r0:r0 + P], t_dst=t_xT))
                return norm_transpose_gen(blocks, xnb, t_xnb)

            drain(prep_1b(0))
            for i in range(OG):
                xT, t_xT = xnT[i % 2], t_xnT[i % 2]
                nxt = prep_1b(i + 1) if i + 1 < OG else None
                qs, t_qs = QTst[i % 2], t_QTst[i % 2]
                for c in range(4):
                    bank, tb = mm_bank()
                    for dc in range(8):
                        op("pe", lambda e, dc=dc, c=c, bank=bank: e.matmul(
                            bank[:, :], lhsT=winPQ[:, dc * 1024 + 512 + c * 128: dc * 1024 + 512 + (c + 1) * 128],
                            rhs=xT[:, dc * 528 + 16: dc * 528 + 528], start=(dc == 0), stop=(dc == 7)),
                           reads=[t_winPQ, t_xT], writes=[tb])
                    op("act", lambda e, c=c, bank=bank: e.activation(out=qs[:, c * 512:(c + 1) * 512], in_=bank[:, :],
                                                                     func=AF.Copy, scale=0.125), reads=[tb], writes=[t_qs])
                    if nxt is not None:
                        next(nxt, None)
                dma("pool", qT_d[i, :, :], qs[:, :], reads=[t_qs], writes=[t_q[i]])
                for pg in range(4):
                    bank, tb = mm_bank()
                    for dc in range(8):
                        op("pe", lambda e, dc=dc, pg=pg, bank=bank: e.matmul(
                            bank[:, :], lhsT=winPQ[:, dc * 1024 + pg * 128: dc * 1024 + (pg + 1) * 128],
                            rhs=xT[:, dc * 528 + 16: dc * 528 + 528], start=(dc == 0), stop=(dc == 7)),
                           reads=[t_winPQ, t_xT], writes=[tb])
                    op("act", lambda e, pg=pg, bank=bank: e.activation(out=uT[:, pg * 528 + 16: pg * 528 + 528], in_=bank[:, :],
                                                                       func=AF.Copy), reads=[tb], writes=[t_uT])
                    if nxt is not None:
                        next(nxt, None)
                bank, tb = mm_bank()
                for pg in range(4):
                    for dc in range(8):
                        op("pe", lambda e, dc=dc, pg=pg, bank=bank: e.matmul(
                            bank[:, pg * 16:(pg + 1) * 16], lhsT=winPQ[:, dc * 1024 + pg * 128: dc * 1024 + (pg + 1) * 128],
                            rhs=xT[:, dc * 528: dc * 528 + 16], start=(dc == 0), stop=(dc == 7)),
                           reads=[t_winPQ, t_xT], writes=[tb])
                op("act", lambda e, bank=bank: e.activation(out=v3(uT, 4)[:, :, 0:16],
                                                            in_=bank[:, 0:64].rearrange("p (c t) -> p c t", c=4),
                                                            func=AF.Copy), reads=[tb], writes=[t_uT])
                cur = [(uT, pg * 528, t_uT) for pg in range(4)]
                lo = [0, 0, 0, 0]
                for si, d in enumerate((1, 2, 4, 8)):
                    for pg in range(si, 4):
                        src, off, tsrc = cur[pg]
                        dst, tdst = (tA[pg], t_tA[pg]) if si % 2 == 0 else (tB[pg], t_tB[pg])
                        a = lo[pg] + d
                        op("dve", lambda e, src=src, off=off, dst=dst, a=a, d=d: e.tensor_tensor(
                            out=dst[:, a:528], in0=src[:, off + a: off + 528], in1=src[:, off + a - d: off + 528 - d],
                            op=ALU.add), reads=[tsrc], writes=[tdst])
                        cur[pg] = (dst, 0, tdst); lo[pg] = a
                for pg in range(4):
                    src, off, tsrc = cur[pg]
                    wdw = 2 ** (pg + 1)
                    dst, tdst = (tB[pg], t_tB[pg]) if src is tA[pg] else (tA[pg], t_tA[pg])
                    if i == 0:
                        op("dve", lambda e, src=src, dst=dst, pg=pg: e.tensor_tensor(
                            out=dst[:, 16:528], in0=src[:, 16:528], in1=invc[:, pg * 512:(pg + 1) * 512], op=ALU.mult),
                           reads=[tsrc, t_invc], writes=[tdst])
                    else:
                        op("dve", lambda e, src=src, dst=dst, wdw=wdw: e.tensor_scalar(
                            out=dst[:, 16:528], in0=src[:, 16:528], scalar1=1.0 / wdw, scalar2=None, op0=ALU.mult),
                           reads=[tsrc], writes=[tdst])
                    op("dve", lambda e, dst=dst, pg=pg: e.tensor_tensor(
                        out=pooled[:, pg * 512:(pg + 1) * 512], in0=dst[:, 16:528],
                        in1=uT[:, pg * 528 + 16: pg * 528 + 528], op=ALU.subtract),
                       reads=[tdst, t_uT], writes=[t_pooled[pg]])
                pst, t_pst = poSt[i % 2], t_poSt[i % 2]
                for pg in range(4):
                    bank, tb = mm_bank()
                    op("pe", lambda e, pg=pg, bank=bank: e.matmul(bank[:, :], lhsT=wpb[:, pg * 128:(pg + 1) * 128],
                                                                  rhs=pooled[:, pg * 512:(pg + 1) * 512], start=True, stop=True),
                       reads=[t_wpb, t_pooled[pg]], writes=[tb])
                    op("act", lambda e, pg=pg, bank=bank: e.activation(out=pst[:, pg * 512:(pg + 1) * 512], in_=bank[:, :],
                                                                       func=AF.Copy, scale=psc[:, pg:pg + 1]),
                       reads=[tb, t_psc], writes=[t_pst])
                dma("pool", poT_d[i, :, :], pst[:, :], reads=[t_pst], writes=[t_po[i]])
                drain(nxt)
            Sc.barrier()
        if stop_after <= 1:
            Sc.barrier()
            return nc

        with ExitStack() as eskv:
            KT = sb(eskv, "KT", [128, 4 * S], BF16)
            Vt = sb(eskv, "Vt", [128, 64 * 512], BF16)
            t_KT = [Tk() for _ in range(NG)]; t_V = [Tk() for _ in range(NG)]
            with ExitStack() as es:
                winKV = sb(es, "winKV", [128, 8 * 1024], BF16); t_winKV = Tk()
                gmix = sb(es, "gmix2", [128, D], F32); t_gmix = Tk()
                xb = [sb(es, "xc%d" % i, [128, D], F32) for i in range(3)]; t_xb = [Tk() for _ in range(3)]
                xnb = [sb(es, "xnc%d" % i, [128, D], BF16) for i in range(2)]; t_xnb = [Tk() for _ in range(2)]
                xnT = [sb(es, "xnTc%d" % i, [128, 8 * 512], BF16) for i in range(2)]; t_xnT = [Tk() for _ in range(2)]
                load_gain(gmix, t_gmix, g_mix)
                load_w3(winKV, t_winKV, w_in, 8, 1024, 2048)
                def prep_1a(g):
                    xT, t_xT = xnT[g % 2], t_xnT[g % 2]
                    xT3 = v3(xT, 8)
                    blocks = []
                    for blk in range(4):
                        r0 = g * 512 + blk * 128
                        kx = rot["x"]; rot["x"] = (kx + 1) % 3
                        blocks.append(dict(
                            load=(lambda kx=kx, r0=r0: dma("sp", xb[kx][:, :], xf[r0:r0 + 128, :], writes=[t_xb[kx]])),
                            xin=xb[kx][:, :], t_x=t_xb[kx], gain=gmix, t_g=t_gmix,
                            dst3=xT3[:, :, blk * 128:(blk + 1) * 128], t_dst=t_xT))
                    return norm_transpose_gen(blocks, xnb, t_xnb)

                drain(prep_1a(0))
                for g in range(NG):
                    xT, t_xT = xnT[g % 2], t_xnT[g % 2]
                    nxt = prep_1a(g + 1) if g + 1 < NG else None
                    for c in range(4):
                        bank, tb = mm_bank()
                        for dc in range(8):
                            op("pe", lambda e, dc=dc, c=c, bank=bank: e.matmul(
                                bank[:, :], lhsT=winKV[:, dc * 1024 + c * 128: dc * 1024 + (c + 1) * 128],
                                rhs=xT[:, dc * 512:(dc + 1) * 512], start=(dc == 0), stop=(dc == 7)),
                               reads=[t_winKV, t_xT], writes=[tb])
                        op("act", lambda e, c=c, bank=bank, g=g: e.activation(
                            out=KT[:, c * S + g * 512: c * S + (g + 1) * 512], in_=bank[:, :], func=AF.Copy),
                           reads=[tb], writes=[t_KT[g]])
                        if nxt is not None:
                            next(nxt, None)
                    for blk in range(4):
                        bank, tb = mm_bank()
                        for dc in range(8):
                            op("pe", lambda e, dc=dc, blk=blk, bank=bank: e.matmul(
                                bank[:, :], lhsT=xT[:, dc * 512 + blk * 128: dc * 512 + (blk + 1) * 128],
                                rhs=winKV[:, dc * 1024 + 512: dc * 1024 + 1024], start=(dc == 0), stop=(dc == 7)),
                               reads=[t_winKV, t_xT], writes=[tb])
                        kb = g * 4 + blk
                        op("dve", lambda e, kb=kb, bank=bank: e.tensor_copy(out=Vt[:, kb * 512:(kb + 1) * 512], in_=bank[:, :]),
                           reads=[tb], writes=[t_V[g]])
                        if nxt is not None and blk == 0:
                            next(nxt, None)
                    drain(nxt)
                Sc.barrier()

            if stop_after >= 3:
                with ExitStack() as es:
                    mk = sb(es, "mk", [128, 8 * 512], BF16); t_mk = Tk()
                    QTg = [sb(es, "QTg%d" % i, [128, 2048], BF16) for i in range(2)]; t_QTg = [Tk() for _ in range(2)]
                    NBUF = 4
                    e_sb = [sb(es, "e_sb%d" % i, [128, 512], F32) for i in range(NBUF)]; t_e = [Tk() for _ in range(NBUF)]
                    sp_sb = [sb(es, "sp_sb%d" % i, [128, 512], BF16) for i in range(NBUF)]; t_sp = [Tk() for _ in range(NBUF)]
                    G_sb = [sb(es, "G_sb%d" % i, [128, 512], F32) for i in range(NBUF)]; t_G = [Tk() for _ in range(NBUF)]
                    W_sb = [sb(es, "W_sb%d" % i, [128, 512], BF16) for i in range(NBUF)]; t_W = [Tk() for _ in range(NBUF)]
                    aTst = [sb(es, "aTst%d" % i, [64, 4096], BF16) for i in range(2)]; t_aTst = [Tk() for _ in range(2)]
                    for m in range(8):
                        dma("pool", mk[:, m * 512:(m + 1) * 512], masks_d[m, :, :], writes=[t_mk], acc=(m > 0))
                    for e_ in range(NE):
                        for hf in range(2):
                            dma("pool", wg16[e_, hf * 512:(hf + 1) * 512, :], w_gate[e_, hf * 512:(hf + 1) * 512, :],
                                writes=[t_wg16[e_]], acc=(hf > 0))
                            dma("pool", wu16[e_, hf * 512:(hf + 1) * 512, :], w_up[e_, hf * 512:(hf + 1) * 512, :],
                                writes=[t_wu16[e_]], acc=(hf > 0))
                            dma("pool", wd16[e_, hf * 256:(hf + 1) * 256, :], w_down[e_, hf * 256:(hf + 1) * 256, :],
                                writes=[t_wd16[e_]], acc=(hf > 0))
                    items = []
                    for i in range(OG):
                        E = 2 * i + 2
                        nkb = 4 * E
                        for hp in range(4):
                            for kb in range(nkb - 1, -1, -1):
                                for hh in range(2):
                                    m = kb - 4 * (E - 2)
                                    c0 = 128 * (m - 4) if m >= 4 else 0
                                    items.append(dict(i=i, hp=hp, kb=kb, hh=hh, first=(kb == nkb - 1), last=(kb == 0),
                                                      m=(m if m >= 0 else None), c0=c0))
                    NI = len(items)
                    zb = [PS[0], PS[1], PS[2], PS[3]]; t_zb = [t_PS[0], t_PS[1], t_PS[2], t_PS[3]]
                    Cb = [PS[4], PS[5]]; t_Cb = [t_PS[4], t_PS[5]]
                    Ob = [PS[6], PS[7]]; t_Ob = [t_PS[6], t_PS[7]]
                    loaded_q = set()

                    def ensure_q(i):
                        if i < OG and i not in loaded_q:
                            loaded_q.add(i)
                            dma("sp", QTg[i % 2][:, :], qT_d[i, :, :], reads=[t_q[i]], writes=[t_QTg[i % 2]])

                    def st_z(n):
                        it = items[n]; b = n % NBUF
                        ensure_q(it["i"])
                        pl = it["hh"] * 64
                        g = it["kb"] // 4
                        q = QTg[it["i"] % 2]
                        msk = it["m"] is not None
                        c0 = it["c0"]
                        op("pe", lambda e: e.matmul(zb[b][:, c0:512], lhsT=KT[pl:pl + 64, it["hp"] * S + it["kb"] * 128: it["hp"] * S + (it["kb"] + 1) * 128],
                                                    rhs=q[pl:pl + 64, it["hp"] * 512 + c0:(it["hp"] + 1) * 512], start=True, stop=(not msk)),
                           reads=[t_KT[g], t_QTg[it["i"] % 2]], writes=[t_zb[b]])
                        if msk:
                            op("pe", lambda e: e.matmul(zb[b][:, c0:512], lhsT=idb[:, :], rhs=mk[:, it["m"] * 512 + c0:(it["m"] + 1) * 512],
                                                        start=False, stop=True), reads=[t_mk, t_c], writes=[t_zb[b]])

                    def st_e(n):
                        b = n % NBUF; c0 = items[n]["c0"]
                        op("act", lambda e: e.activation(out=e_sb[b][:, c0:512], in_=zb[b][:, c0:512], func=AF.Exp),
                           reads=[t_zb[b]], writes=[t_e[b]])

                    def st_sp(n):
                        b = n % NBUF; c0 = items[n]["c0"]
                        op("act", lambda e: e.activation(out=sp_sb[b][:, c0:512], in_=e_sb[b][:, c0:512], func=AF.Ln, bias=1.0),
                           reads=[t_e[b]], writes=[t_sp[b]])

                    def st_U(n):
                        it = items[n]; b = n % NBUF; h = it["hh"]
                        c0 = it["c0"]
                        op("pe", lambda e: e.matmul(Cb[h][:, c0:512], lhsT=uib[:, :], rhs=sp_sb[b][:, c0:512], start=it["first"], stop=True,
                                                    skip_group_check=True),
                           reads=[t_sp[b], t_c], writes=[t_Cb[h]])

                    def st_G(n):
                        it = items[n]; b = n % NBUF; h = it["hh"]
                        c0 = it["c0"]
                        op("act", lambda e: e.activation(out=G_sb[b][:, c0:512], in_=Cb[h][:, c0:512], func=AF.Exp, scale=-1.0),
                           reads=[t_Cb[h]], writes=[t_G[b]])

                    def st_L(n):
                        it = items[n]; b = n % NBUF; h = it["hh"]
                        c0 = it["c0"]
                        op("pe", lambda e: e.matmul(Cb[h][:, c0:512], lhsT=lsb[:, :], rhs=sp_sb[b][:, c0:512], start=False, stop=True,
                                                    skip_group_check=True),
                           reads=[t_sp[b], t_c], writes=[t_Cb[h]])

                    def st_W(n):
                        b = n % NBUF; c0 = items[n]["c0"]
                        op("dve", lambda e: e.tensor_tensor(out=W_sb[b][:, c0:512], in0=e_sb[b][:, c0:512], in1=G_sb[b][:, c0:512], op=ALU.mult),
                           reads=[t_e[b], t_G[b]], writes=[t_W[b]])

                    def st_PV(n):
                        it = items[n]; b = n % NBUF; h = it["hh"]
                        H = it["hp"] * 2 + h
                        g = it["kb"] // 4
                        c0 = it["c0"]
                        op("pe", lambda e: e.matmul(Ob[h][0:64, c0:512], lhsT=Vt[:, it["kb"] * 512 + H * 64: it["kb"] * 512 + (H + 1) * 64],
                                                    rhs=W_sb[b][:, c0:512], start=it["first"], stop=True, skip_group_check=True),
                           reads=[t_V[g], t_W[b]], writes=[t_Ob[h]])
                        if it["last"]:
                            ast, t_ast = aTst[it["i"] % 2], t_aTst[it["i"] % 2]
                            op("dve", lambda e: e.tensor_copy(out=ast[0:64, H * 512:(H + 1) * 512], in_=Ob[h][0:64, :]),
                               reads=[t_Ob[h]], writes=[t_ast])
                            if H == 7:
                                dma("sp", aT_d[it["i"], :, :], ast[:, :], reads=[t_ast], writes=[t_a[it["i"]]])

                    for n in range(-3, NI + 1):
                        if 0 <= n + 3 < NI:
                            st_z(n + 3)
                            st_e(n + 3)
                        if 0 <= n + 2 < NI:
                            st_sp(n + 2)
                        if 0 <= n + 1 < NI:
                            st_U(n + 1)
                            st_G(n + 1)
                        if 0 <= n < NI:
                            st_L(n)
                            st_W(n)
                        if 0 <= n - 1 < NI:
                            st_PV(n - 1)
                    Sc.barrier()
        if stop_after <= 3:
            Sc.barrier()
            return nc

        with ExitStack() as es:
            woP = sb(es, "woP", [128, 4 * 1024], BF16); t_woP = Tk()
            woA = sb(es, "woA", [128, 4 * 1024], BF16); t_woA = Tk()
            poT = [sb(es, "poT%d" % i, [128, 2048], BF16) for i in range(2)]; t_poT = [Tk() for _ in range(2)]
            aT = [sb(es, "aT%d" % i, [128, 2048], BF16) for i in range(2)]; t_aT = [Tk() for _ in range(2)]
            xb = [sb(es, "xd%d" % i, [128, D], F32) for i in range(3)]; t_xb = [Tk() for _ in range(3)]
            hst = [sb(es, "hst%d" % i, [128, D], F32) for i in range(3)]; t_hst = [Tk() for _ in range(3)]
            load_w3(woP, t_woP, w_out, 4, 0, 1024)
            load_w3(woA, t_woA, w_out[512:1024, :], 4, 0, 1024)
            kk = 0

            def load_mix(i):
                dma("sp", poT[i % 2][:, :], poT_d[i, :, :], reads=[t_po[i]], writes=[t_poT[i % 2]])
                for h in range(8):
                    dma("sp", aT[i % 2][(h % 2) * 64:(h % 2) * 64 + 64, (h // 2) * 512:(h // 2 + 1) * 512],
                        aT_d[i, :, h * 512:(h + 1) * 512], reads=[t_a[i]], writes=[t_aT[i % 2]], acc=(h > 0))

            load_mix(0)
            for i in range(OG):
                if i + 1 < OG:
                    load_mix(i + 1)
                p_, a_ = poT[i % 2], aT[i % 2]
                for blk in range(4):
                    kx = kk % 3; kk += 1
                    dma("sp", xb[kx][:, :], xo[i, 16 + blk * 128: 16 + (blk + 1) * 128, :], writes=[t_xb[kx]])
                    for half in range(2):
                        bank, tb = mm_bank()
                        for pg in range(4):
                            op("pe", lambda e, pg=pg, bank=bank, blk=blk, half=half: e.matmul(
                                bank[:, :], lhsT=p_[:, pg * 512 + blk * 128: pg * 512 + (blk + 1) * 128],
                                rhs=woP[:, pg * 1024 + half * 512: pg * 1024 + (half + 1) * 512], start=(pg == 0), stop=False),
                               reads=[t_woP, t_poT[i % 2]], writes=[tb])
                        for h in range(4):
                            op("pe", lambda e, h=h, bank=bank, blk=blk, half=half: e.matmul(
                                bank[:, :], lhsT=a_[:, h * 512 + blk * 128: h * 512 + (blk + 1) * 128],
                                rhs=woA[:, h * 1024 + half * 512: h * 1024 + (half + 1) * 512], start=False, stop=(h == 3)),
                               reads=[t_woA, t_aT[i % 2]], writes=[tb])
                        op("dve", lambda e, bank=bank, half=half, kx=kx: e.tensor_tensor(
                            out=hst[kx][:, half * 512:(half + 1) * 512], in0=bank[:, :], in1=xb[kx][:, half * 512:(half + 1) * 512],
                            op=ALU.add), reads=[tb, t_xb[kx]], writes=[t_hst[kx]])
                    dma("pool", h1_d[i * 512 + blk * 128: i * 512 + (blk + 1) * 128, :], hst[kx][:, :],
                        reads=[t_hst[kx]], writes=[t_h1[i * 4 + blk]])
            Sc.barrier()
        if stop_after <= 4:
            Sc.barrier()
            return nc

        with ExitStack() as es:
            KmT = sb(es, "KmT", [128, 8 * 256], BF16); t_KmT = Tk()
            Vm = sb(es, "Vm", [128, 2 * 1024], BF16); t_Vm = Tk()
            wqb = sb(es, "wqb", [128, 8 * 1024], BF16); t_wqb = Tk()
            wob = sb(es, "wob", [128, 8 * 1024], BF16); t_wob = Tk()
            gcr = sb(es, "gcr", [128, D], F32); t_gcr = Tk()
            load_gain(gcr, t_gcr, g_cross)
            load_w3(wqb, t_wqb, wq, 8, 0, 1024)
            load_w3(wob, t_wob, wo, 8, 0, 1024)
            xnb = [sb(es, "xne%d" % i, [128, D], BF16) for i in range(2)]; t_xnb = [Tk() for _ in range(2)]
            with ExitStack() as es2:
                wkb = sb(es2, "wkb", [128, 8 * 1024], BF16); t_wkb = Tk()
                wvb = sb(es2, "wvb", [128, 8 * 1024], BF16); t_wvb = Tk()
                gme = sb(es2, "gme", [128, D], F32); t_gme = Tk()
                mb_ = [sb(es2, "memb%d" % i, [128, D], F32) for i in range(2)]; t_mb = [Tk() for _ in range(2)]
                memT = sb(es2, "memT", [128, 8 * 256], BF16); t_memT = Tk()
                load_gain(gme, t_gme, g_mem)
                load_w3(wkb, t_wkb, wk, 8, 0, 1024)
                load_w3(wvb, t_wvb, wv, 8, 0, 1024)
                for k in range(2):
                    dma("sp", mb_[k][:, :], mem[k * 128:(k + 1) * 128, :], writes=[t_mb[k]])
                    norm_block(mb_[k][:, :], t_mb[k], gme, t_gme, xnb[k][:, :], t_xnb[k])
                    transpose_block(xnb[k], t_xnb[k], v3(memT, 8)[:, :, k * 128:(k + 1) * 128], t_memT)
                for ec in range(8):
                    bank, tb = mm_bank()
                    for dc in range(8):
                        op("pe", lambda e, dc=dc, ec=ec, bank=bank: e.matmul(
                            bank[:, 0:256], lhsT=wkb[:, dc * 1024 + ec * 128: dc * 1024 + (ec + 1) * 128],
                            rhs=memT[:, dc * 256:(dc + 1) * 256], start=(dc == 0), stop=(dc == 7)),
                           reads=[t_wkb, t_memT], writes=[tb])
                    op("act", lambda e, ec=ec, bank=bank: e.activation(out=KmT[:, ec * 256:(ec + 1) * 256], in_=bank[:, 0:256], func=AF.Copy),
                       reads=[tb], writes=[t_KmT])
                for k in range(2):
                    for half in range(2):
                        bank, tb = mm_bank()
                        for dc in range(8):
                            op("pe", lambda e, dc=dc, k=k, half=half, bank=bank: e.matmul(
                                bank[:, :], lhsT=memT[:, dc * 256 + k * 128: dc * 256 + (k + 1) * 128],
                                rhs=wvb[:, dc * 1024 + half * 512: dc * 1024 + (half + 1) * 512], start=(dc == 0), stop=(dc == 7)),
                               reads=[t_wvb, t_memT], writes=[tb])
                        op("act", lambda e, k=k, half=half, bank=bank: e.activation(
                            out=Vm[:, k * 1024 + half * 512: k * 1024 + (half + 1) * 512], in_=bank[:, :], func=AF.Copy),
                           reads=[tb], writes=[t_Vm])
                Sc.barrier()
            h1g = [sb(es, "h1g%d" % i, [128, 4 * D], F32) for i in range(2)]; t_h1g = [[Tk() for _ in range(4)] for _ in range(2)]
            xcT = [sb(es, "xcT%d" % i, [128, 8 * 512], BF16) for i in range(2)]; t_xcT = [Tk() for _ in range(2)]
            qmT = sb(es, "qmT", [128, 8 * 512], BF16); t_qmT = Tk()
            pE = [sb(es, "pE%d" % i, [128, 256], F32) for i in range(4)]; t_pE = [Tk() for _ in range(4)]
            pn = [sb(es, "pn%d" % i, [128, 256], BF16) for i in range(4)]; t_pn = [Tk() for _ in range(4)]
            sm = [sb(es, "sm%d" % i, [128, 4], F32) for i in range(8)]; t_sm = [Tk() for _ in range(8)]
            pTa = sb(es, "pTa", [128, 8 * 512], BF16); t_pTa = Tk()
            oT = sb(es, "oT", [128, 8 * 512], BF16); t_oT = Tk()
            hst = [sb(es, "hse%d" % i, [128, D], F32) for i in range(2)]; t_hst = [Tk() for _ in range(2)]
            ks = 0; kh = 0

            def prep_3c(i):
                hg, thg = h1g[i % 2], t_h1g[i % 2]
                xc3 = v3(xcT[i % 2], 8)
                blocks = []
                for blk in range(4):
                    blocks.append(dict(
                        load=(lambda blk=blk, hg=hg, thg=thg, i=i: dma(
                            "sp", hg[:, blk * D:(blk + 1) * D], h1_d[i * 512 + blk * 128: i * 512 + (blk + 1) * 128, :],
                            reads=[t_h1[i * 4 + blk]], writes=[thg[blk]])),
                        xin=hg[:, blk * D:(blk + 1) * D], t_x=thg[blk], gain=gcr, t_g=t_gcr,
                        dst3=xc3[:, :, blk * 128:(blk + 1) * 128], t_dst=t_xcT[i % 2]))
                return norm_transpose_gen(blocks, xnb, t_xnb)

            drain(prep_3c(0))
            for i in range(OG):
                hg, thg = h1g[i % 2], t_h1g[i % 2]
                xc, t_xc = xcT[i % 2], t_xcT[i % 2]
                nxt = prep_3c(i + 1) if i + 1 < OG else None
                for ec in range(8):
                    bank, tb = mm_bank()
                    for dc in range(8):
                        op("pe", lambda e, dc=dc, ec=ec, bank=bank: e.matmul(
                            bank[:, :], lhsT=wqb[:, dc * 1024 + ec * 128: dc * 1024 + (ec + 1) * 128],
                            rhs=xc[:, dc * 512:(dc + 1) * 512], start=(dc == 0), stop=(dc == 7)),
                           reads=[t_wqb, t_xc], writes=[tb])
                    op("act", lambda e, ec=ec, bank=bank: e.activation(out=qmT[:, ec * 512:(ec + 1) * 512], in_=bank[:, :],
                                                                       func=AF.Copy, scale=1.0 / 16.0), reads=[tb], writes=[t_qmT])
                for blk in range(4):
                    banks = [mm_bank() for _ in range(4)]
                    smt = []
                    for hh in range(4):
                        bank, tb = banks[hh]
                        for c2 in range(2):
                            ec = hh * 2 + c2
                            op("pe", lambda e, ec=ec, c2=c2, blk=blk, bank=bank: e.matmul(
                                bank[:, 0:256], lhsT=qmT[:, ec * 512 + blk * 128: ec * 512 + (blk + 1) * 128],
                                rhs=KmT[:, ec * 256:(ec + 1) * 256], start=(c2 == 0), stop=(c2 == 1)),
                               reads=[t_qmT, t_KmT], writes=[tb])
                        smt.append((sm[ks % 8], t_sm[ks % 8])); ks += 1
                    for hh in range(4):
                        bank, tb = banks[hh]; s_, ts_ = smt[hh]
                        op("dve", lambda e, bank=bank, s_=s_: e.tensor_reduce(out=s_[:, 0:1], in_=bank[:, 0:256], axis=AX.X, op=ALU.max),
                           reads=[tb], writes=[ts_])
                    for hh in range(4):
                        s_, ts_ = smt[hh]
                        op("dve", lambda e, s_=s_: e.tensor_scalar(out=s_[:, 1:2], in0=s_[:, 0:1], scalar1=-1.0, scalar2=None, op0=ALU.mult),
                           reads=[ts_], writes=[ts_])
                    for hh in range(4):
                        bank, tb = banks[hh]; s_, ts_ = smt[hh]
                        op("act", lambda e, bank=bank, s_=s_, hh=hh: e.activation(out=pE[hh][:, :], in_=bank[:, 0:256], func=AF.Exp,
                                                                                 bias=s_[:, 1:2], accum_out=s_[:, 2:3]),
                           reads=[tb, ts_], writes=[t_pE[hh], ts_])
                    for hh in range(4):
                        s_, ts_ = smt[hh]
                        op("dve", lambda e, s_=s_: e.reciprocal(out=s_[:, 3:4], in_=s_[:, 2:3]), reads=[ts_], writes=[ts_])
                    for hh in range(4):
                        s_, ts_ = smt[hh]
                        op("dve", lambda e, s_=s_, hh=hh: e.tensor_scalar(out=pn[hh][:, :], in0=pE[hh][:, :], scalar1=s_[:, 3:4],
                                                                         scalar2=None, op0=ALU.mult),
                           reads=[t_pE[hh], ts_], writes=[t_pn[hh]])
                    k = rot["tp"]; rot["tp"] = (k + 1) % 2
                    tbank, ttb = PS[k], t_PS[k]
                    psb = tbank[:, :].bitcast(BF16)
                    for hh in range(4):
                        for mb2 in range(2):
                            j = hh * 2 + mb2
                            op("pe", lambda e, mb2=mb2, hh=hh, j=j, psb=psb: e.transpose(
                                out=psb[:, j * 128:(j + 1) * 128], in_=pn[hh][:, mb2 * 128:(mb2 + 1) * 128], identity=idb[:, :]),
                               reads=[t_pn[hh], t_c], writes=[ttb])
                    op("act", lambda e, psb=psb, blk=blk: e.activation(
                        out=v3(pTa, 8)[:, :, blk * 128:(blk + 1) * 128], in_=psb[:, 0:1024].rearrange("p (c t) -> p c t", c=8),
                        func=AF.Copy), reads=[ttb], writes=[t_pTa])
                    if nxt is not None:
                        next(nxt, None)
                for ec in range(8):
                    hh = ec // 2
                    bank, tb = mm_bank()
                    for mb2 in range(2):
                        op("pe", lambda e, mb2=mb2, ec=ec, hh=hh, bank=bank: e.matmul(
                            bank[:, :], lhsT=Vm[:, mb2 * 1024 + ec * 128: mb2 * 1024 + (ec + 1) * 128],
                            rhs=pTa[:, (hh * 2 + mb2) * 512:(hh * 2 + mb2 + 1) * 512], start=(mb2 == 0), stop=(mb2 == 1)),
                           reads=[t_Vm, t_pTa], writes=[tb])
                    op("act", lambda e, ec=ec, bank=bank: e.activation(out=oT[:, ec * 512:(ec + 1) * 512], in_=bank[:, :], func=AF.Copy),
                       reads=[tb], writes=[t_oT])
                    if nxt is not None and ec == 0:
                        next(nxt, None)
                for blk in range(4):
                    kx = kh % 2; kh += 1
                    for half in range(2):
                        bank, tb = mm_bank()
                        for ec in range(8):
                            op("pe", lambda e, ec=ec, bank=bank, blk=blk, half=half: e.matmul(
                                bank[:, :], lhsT=oT[:, ec * 512 + blk * 128: ec * 512 + (blk + 1) * 128],
                                rhs=wob[:, ec * 1024 + half * 512: ec * 1024 + (half + 1) * 512], start=(ec == 0), stop=(ec == 7)),
                               reads=[t_wob, t_oT], writes=[tb])
                        op("dve", lambda e, bank=bank, half=half, kx=kx, blk=blk, hg=hg: e.tensor_tensor(
                            out=hst[kx][:, half * 512:(half + 1) * 512], in0=bank[:, :],
                            in1=hg[:, blk * D + half * 512: blk * D + (half + 1) * 512], op=ALU.add),
                           reads=[tb, thg[blk]], writes=[t_hst[kx]])
                    dma("pool", h2_d[i * 512 + blk * 128: i * 512 + (blk + 1) * 128, :], hst[kx][:, :],
                        reads=[t_hst[kx]], writes=[t_h2[i * 4 + blk]])
                drain(nxt)
            Sc.barrier()
        if stop_after <= 5:
            Sc.barrier()
            return nc

        with ExitStack() as es:
            TB = 8
            NT = OG * 4 // TB
            TW = TB * 128
            hacc = [sb(es, "hacc%d" % i, [128, TB * D], F32) for i in range(2)]
            t_hacc = [[Tk() for _ in range(TB)] for _ in range(2)]
            xfT = [sb(es, "xfT%d" % i, [128, 8 * TW], BF16) for i in range(2)]; t_xfT = [Tk() for _ in range(2)]
            comb = [sb(es, "comb%d" % i, [128, TB * 16], F32) for i in range(2)]
            t_comb = [[Tk() for _ in range(TB)] for _ in range(2)]
            rt = [sb(es, "rt%d" % i, [128, 64], F32) for i in range(16)]; t_rt = [Tk() for _ in range(16)]
            xnb = [sb(es, "xnf%d" % i, [128, D], BF16) for i in range(2)]; t_xnb = [Tk() for _ in range(2)]
            gff = sb(es, "gff", [128, D], F32); t_gff = Tk()
            gfn = sb(es, "gfn", [128, D], F32); t_gfn = Tk()
            wgeb = sb(es, "wgeb", [128, 8 * 20], BF16); t_wgeb = Tk()
            wgb = [sb(es, "wgb%d" % i, [128, 8 * FF], BF16) for i in range(2)]; t_wgb = [Tk() for _ in range(2)]
            wub = [sb(es, "wub%d" % i, [128, 8 * FF], BF16) for i in range(2)]; t_wub = [Tk() for _ in range(2)]
            wdb = [sb(es, "wdb%d" % i, [128, 4 * D], BF16) for i in range(1)]; t_wdb = [Tk() for _ in range(1)]
            hmT = [sb(es, "hmT%d" % i, [128, 4 * TW], BF16) for i in range(1)]; t_hmT = [Tk() for _ in range(1)]
            sg = [sb(es, "sg%d" % i, [128, 512], F32) for i in range(2)]; t_sg = [Tk() for _ in range(2)]
            ost = [sb(es, "ost%d" % i, [128, D], F32) for i in range(2)]; t_ost = [Tk() for _ in range(2)]
            load_gain(gff, t_gff, g_ffn)
            load_gain(gfn, t_gfn, g_fin)
            load_w3(wgeb, t_wgeb, w_ge, 8, 0, 20)
            cnt = {"sg": 0, "ost": 0}

            def load_gu(e_):
                p = e_ % 2
                for dc in range(8):
                    dma("pool", wgb[p][:, dc * FF:(dc + 1) * FF], wg16[e_, dc * 128:(dc + 1) * 128, :], reads=[t_wg16[e_]], writes=[t_wgb[p]], acc=(dc > 0))
                for dc in range(8):
                    dma("pool", wub[p][:, dc * FF:(dc + 1) * FF], wu16[e_, dc * 128:(dc + 1) * 128, :], reads=[t_wu16[e_]], writes=[t_wub[p]], acc=(dc > 0))

            def load_d(e_):
                for fc in range(4):
                    dma("pool", wdb[0][:, fc * D:(fc + 1) * D], wd16[e_, fc * 128:(fc + 1) * 128, :], reads=[t_wd16[e_]], writes=[t_wdb[0]], acc=(fc > 0))

            def prologue_gen(ti):
                bs = ti % 2
                hac, thac, xT, t_xT, cmb, t_cmb = hacc[bs], t_hacc[bs], xfT[bs], t_xfT[bs], comb[bs], t_comb[bs]
                blocks = []
                for blk in range(TB):
                    gb = ti * TB + blk
                    hb = hac[:, blk * D:(blk + 1) * D]
                    blocks.append(dict(
                        load=(lambda hb=hb, gb=gb, blk=blk: dma("sp", hb, h2_d[gb * 128:(gb + 1) * 128, :],
                                                                 reads=[t_h2[gb]], writes=[thac[blk]])),
                        xin=hb, t_x=thac[blk], gain=gff, t_g=t_gff,
                        dst3=v3(xT, 8)[:, :, blk * 128:(blk + 1) * 128], t_dst=t_xT))
                yield from norm_transpose_gen(blocks, xnb, t_xnb)
                RB = []
                for blk in range(TB):
                    bank, tb = mm_bank()
                    for dc in range(8):
                        op("pe", lambda e, dc=dc, bank=bank, blk=blk: e.matmul(
                            bank[:, 0:20], lhsT=xT[:, dc * TW + blk * 128: dc * TW + (blk + 1) * 128],
                            rhs=wgeb[:, dc * 20:(dc + 1) * 20], start=(dc == 0), stop=(dc == 7)),
                           reads=[t_xT, t_wgeb], writes=[tb])
                    R, tR = rt[bs * 8 + blk], t_rt[bs * 8 + blk]
                    op("dve", lambda e, bank=bank, R=R: e.tensor_copy(out=R[:, 0:20], in_=bank[:, 0:20]), reads=[tb], writes=[tR])
                    RB.append((R, tR))
                    yield
                stages = [
                    ("dve", lambda e, R, blk: e.tensor_reduce(out=R[:, 20:21], in_=R[:, 0:4], axis=AX.X, op=ALU.max)),
                    ("dve", lambda e, R, blk: e.tensor_scalar(out=R[:, 21:22], in0=R[:, 20:21], scalar1=-1.0, scalar2=None, op0=ALU.mult)),
                    ("act", lambda e, R, blk: e.activation(out=R[:, 57:61], in_=R[:, 0:4], func=AF.Exp, bias=R[:, 21:22], accum_out=R[:, 22:23])),
                    ("dve", lambda e, R, blk: e.reciprocal(out=R[:, 23:24], in_=R[:, 22:23])),
                    ("dve", lambda e, R, blk: e.tensor_scalar(out=R[:, 24:28], in0=R[:, 0:4], scalar1=R[:, 20:21], scalar2=None, op0=ALU.is_ge)),
                    ("dve", lambda e, R, blk: e.tensor_scalar(out=R[:, 28:32], in0=R[:, 4:8], scalar1=R[:, 24:25], scalar2=None, op0=ALU.mult)),
                ]
                for g_ in range(1, 4):
                    stages.append(("dve", lambda e, R, blk, g_=g_: e.scalar_tensor_tensor(
                        out=R[:, 28:32], in0=R[:, 4 + 4 * g_: 8 + 4 * g_], scalar=R[:, 24 + g_: 25 + g_], in1=R[:, 28:32], op0=ALU.mult, op1=ALU.add)))
                stages += [
                    ("dve", lambda e, R, blk: e.tensor_reduce(out=R[:, 32:33], in_=R[:, 28:32], axis=AX.X, op=ALU.max)),
                    ("dve", lambda e, R, blk: e.tensor_scalar(out=R[:, 33:37], in0=R[:, 28:32], scalar1=R[:, 32:33], scalar2=None, op0=ALU.is_ge)),
                    ("dve", lambda e, R, blk: e.scalar_tensor_tensor(out=R[:, 37:41], in0=R[:, 33:37], scalar=-1e30, in1=R[:, 28:32], op0=ALU.mult, op1=ALU.add)),
                    ("dve", lambda e, R, blk: e.tensor_reduce(out=R[:, 41:42], in_=R[:, 37:41], axis=AX.X, op=ALU.max)),
                    ("dve", lambda e, R, blk: e.tensor_scalar(out=R[:, 42:46], in0=R[:, 37:41], scalar1=R[:, 41:42], scalar2=None, op0=ALU.is_ge)),
                    ("dve", lambda e, R, blk: e.tensor_tensor(out=R[:, 46:47], in0=R[:, 41:42], in1=R[:, 32:33], op=ALU.subtract)),
                    ("act", lambda e, R, blk: e.activation(out=R[:, 47:48], in_=R[:, 46:47], func=AF.Exp)),
                    ("dve", lambda e, R, blk: e.tensor_scalar(out=R[:, 47:48], in0=R[:, 47:48], scalar1=1.0, scalar2=None, op0=ALU.add)),
                    ("dve", lambda e, R, blk: e.reciprocal(out=R[:, 47:48], in_=R[:, 47:48])),
                    ("dve", lambda e, R, blk: e.tensor_scalar(out=R[:, 48:49], in0=R[:, 47:48], scalar1=-1.0, scalar2=1.0, op0=ALU.mult, op1=ALU.add)),
                    ("dve", lambda e, R, blk: e.tensor_scalar(out=R[:, 49:53], in0=R[:, 33:37], scalar1=R[:, 47:48], scalar2=None, op0=ALU.mult)),
                    ("dve", lambda e, R, blk: e.scalar_tensor_tensor(out=R[:, 49:53], in0=R[:, 42:46], scalar=R[:, 48:49], in1=R[:, 49:53], op0=ALU.mult, op1=ALU.add)),
                    ("dve", lambda e, R, blk: e.tensor_scalar(out=R[:, 53:57], in0=R[:, 24:28], scalar1=R[:, 23:24], scalar2=None, op0=ALU.mult)),
                ]
                for eng, fn in stages:
                    for blk in range(TB):
                        R, tR = RB[blk]
                        op(eng, (lambda e, R=R, blk=blk, fn=fn: fn(e, R, blk)), reads=[tR], writes=[tR])
                    yield
                for g_ in range(4):
                    for blk in range(TB):
                        R, tR = RB[blk]
                        op("dve", lambda e, g_=g_, blk=blk, R=R: e.tensor_scalar(out=cmb[:, blk * 16 + g_ * 4: blk * 16 + g_ * 4 + 4], in0=R[:, 49:53],
                                                                                 scalar1=R[:, 53 + g_: 54 + g_], scalar2=None, op0=ALU.mult),
                           reads=[tR], writes=[t_cmb[blk]])
                    yield

            def epilogue_gen(ti):
                bs = ti % 2
                for blk in range(TB):
                    gb = ti * TB + blk
                    ko = cnt["ost"] % 2; cnt["ost"] += 1
                    hb = hacc[bs][:, blk * D:(blk + 1) * D]
                    norm_block(hb, t_hacc[bs][blk], gfn, t_gfn, ost[ko][:, :], t_ost[ko])
                    dma("pool", y[gb * 128:(gb + 1) * 128, :], ost[ko][:, :], reads=[t_ost[ko]], writes=[Tk()])
                    yield

            def chain(*gens):
                for g_ in gens:
                    if g_ is not None:
                        yield from g_

            def expert(ti, e_, tick):
                bs = ti % 2
                p = e_ % 2
                xT, t_xT = xfT[bs], t_xfT[bs]
                hm, thm = hmT[0], t_hmT[0]
                for th in range(TB // 4):
                    for fc in range(4):
                        bg, tbg = mm_bank()
                        bu, tbu = mm_bank()
                        for dc in range(8):
                            op("pe", lambda e, dc=dc, fc=fc, th=th, bg=bg: e.matmul(
                                bg[:, :], lhsT=wgb[p][:, dc * FF + fc * 128: dc * FF + (fc + 1) * 128],
                                rhs=xT[:, dc * TW + th * 512: dc * TW + (th + 1) * 512], start=(dc == 0), stop=(dc == 7)),
                               reads=[t_wgb[p], t_xT], writes=[tbg])
                        for dc in range(8):
                            op("pe", lambda e, dc=dc, fc=fc, th=th, bu=bu: e.matmul(
                                bu[:, :], lhsT=wub[p][:, dc * FF + fc * 128: dc * FF + (fc + 1) * 128],
                                rhs=xT[:, dc * TW + th * 512: dc * TW + (th + 1) * 512], start=(dc == 0), stop=(dc == 7)),
                               reads=[t_wub[p], t_xT], writes=[tbu])
                        s_, ts_ = sg[cnt["sg"] % 2], t_sg[cnt["sg"] % 2]; cnt["sg"] += 1
                        op("act", lambda e, bg=bg, s_=s_: e.activation(out=s_[:, :], in_=bg[:, :], func=AF.Silu),
                           reads=[tbg], writes=[ts_])
                        op("dve", lambda e, bu=bu, s_=s_, fc=fc, th=th, hm=hm: e.tensor_tensor(
                            out=hm[:, fc * TW + th * 512: fc * TW + (th + 1) * 512], in0=bu[:, :], in1=s_[:, :], op=ALU.mult),
                           reads=[tbu, ts_], writes=[thm])
                        tick()
                for blk in range(TB):
                    for half in range(2):
                        bank, tb = mm_bank()
                        for fc in range(4):
                            op("pe", lambda e, fc=fc, blk=blk, half=half, bank=bank, hm=hm: e.matmul(
                                bank[:, :], lhsT=hm[:, fc * TW + blk * 128: fc * TW + (blk + 1) * 128],
                                rhs=wdb[0][:, fc * D + half * 512: fc * D + (half + 1) * 512], start=(fc == 0), stop=(fc == 3)),
                               reads=[t_wdb[0], thm], writes=[tb])
                        hb = hacc[bs][:, blk * D + half * 512: blk * D + (half + 1) * 512]
                        op("dve", lambda e, bank=bank, hb=hb, blk=blk, e_=e_: e.scalar_tensor_tensor(
                            out=hb, in0=bank[:, :], scalar=comb[bs][:, blk * 16 + e_: blk * 16 + e_ + 1], in1=hb, op0=ALU.mult, op1=ALU.add),
                           reads=[tb, t_comb[bs][blk], t_hacc[bs][blk]], writes=[t_hacc[bs][blk]])
                        tick()

            load_gu(0)
            load_d(0)
            drain(prologue_gen(0))
            for ti in range(NT):
                side = chain(epilogue_gen(ti - 1) if ti > 0 else None,
                             prologue_gen(ti + 1) if ti + 1 < NT else None)
                state = {"n": 0}

                def tick(side=side, state=state):
                    state["n"] += 1
                    if state["n"] % 5 == 0:
                        next(side, None)

                for e_ in range(NE):
                    nxt_e = (e_ + 1) % NE
                    if not (ti == NT - 1 and e_ == NE - 1):
                        load_gu(nxt_e)
                    expert(ti, e_, tick)
                    if not (ti == NT - 1 and e_ == NE - 1):
                        load_d(nxt_e)
                drain(side)
            drain(epilogue_gen(NT - 1))
            Sc.barrier()
        Sc.barrier()
    return nc


_NC_CACHE = {}


def _consts(r):
    s = np.arange(128)[:, None]
    t = np.arange(512)[None, :]
    Dm = [((128 * jj + s) < t).astype(np.float32) for jj in range(4)]
    Z = np.zeros((128, 512), np.float32)
    O = np.ones((128, 512), np.float32)
    masks = np.stack(Dm + [Z] * 4) if r == 0 else np.stack([O] * 4 + Dm)
    masks = (masks - 1.0) * 30000.0
    inv = np.zeros((128, 4, 512), np.float32)
    for pg in range(4):
        w = 2 ** (pg + 1)
        if r == 0:
            inv[:, pg, :] = 1.0 / np.minimum(np.arange(512) + 1, w)[None, :]
        else:
            inv[:, pg, :] = 1.0 / w
    j = np.arange(128)[:, None]
    c = np.arange(128)[None, :]
    ui = (j >= c).astype(np.float32)
    ls = (j < c).astype(np.float32)
    return masks, inv.reshape(128, 2048), ui, ls


def make_in_maps(inp):
    f = lambda a: np.ascontiguousarray(np.asarray(a, dtype=np.float32))
    x = f(inp["x"]); memv = f(inp["mem"])
    shared = dict(
        w_in=f(inp["w_in"][0]), w_pool=f(inp["w_pool"][0]),
        pscale=f(np.asarray(inp["pool_scale"][0]).reshape(4, 128).T),
        w_out=f(inp["w_out"][0]), g_mix=f(inp["norm_mix"][0]), g_cross=f(inp["norm_cross"][0]),
        g_mem=f(inp["norm_mem"][0]), g_ffn=f(inp["norm_ffn"][0]), g_fin=f(inp["norm_final"]),
        wq=f(inp["w_q_mem"][0]), wk=f(inp["w_k_mem"][0]), wv=f(inp["w_v_mem"][0]), wo=f(inp["w_o_mem"][0]),
        w_ge=f(np.concatenate([np.asarray(inp["w_group"][0]), np.asarray(inp["w_expert"][0])], axis=1)),
        w_gate=f(inp["w_gate"][0]), w_up=f(inp["w_up"][0]), w_down=f(inp["w_down"][0]),
        ident=np.eye(128, dtype=np.float32),
    )
    maps = []
    for c in range(8):
        b, r = c // 2, c % 2
        masks, inv, ui, ls = _consts(r)
        xo = np.zeros((OG, 528, D), np.float32)
        for i in range(OG):
            g = 2 * i + r
            s0 = g * 512
            if s0 >= 16:
                xo[i] = x[b, s0 - 16:s0 + 512]
            else:
                xo[i, 16:] = x[b, 0:512]
        m = dict(shared)
        m.update(xf=x[b], xo=xo, mem=memv[b], masks=masks, invcnt=inv, ui=ui, ls=ls)
        maps.append(m)
    return maps


def kernel(**inputs):
    if "nc" not in _NC_CACHE:
        _NC_CACHE["nc"] = build()
    nc = _NC_CACHE["nc"]
    maps = make_in_maps(inputs)
    res = run_bass_kernel_spmd(nc, maps, core_ids=list(range(8)))
    out = np.zeros((4, S, D), np.float32)
    for c in range(8):
        b, r = c // 2, c % 2
        yc = np.asarray(res.results[c]["y"]).reshape(OG, 512, D)
        for i in range(OG):
            g = 2 * i + r
            out[b, g * 512:(g + 1) * 512] = yc[i]
    return out
```

```python
import numpy as np
from contextlib import ExitStack
import concourse.bass as bass
import concourse.mybir as mybir
from concourse.bass_utils import run_bass_kernel_spmd

F32 = mybir.dt.float32
BF16 = mybir.dt.bfloat16
AF = mybir.ActivationFunctionType
ALU = mybir.AluOpType
AX = mybir.AxisListType

D = 1024
S = 8192
NG = 16
OG = 8
NE = 16
FF = 512
EPS = 1e-6


class Tk:
    __slots__ = ("w", "r")

    def __init__(self):
        self.w = []
        self.r = {}


class Sched:
    def __init__(self, nc, ndma=16):
        self.nc = nc
        self.act = {}
        for name, eng in (("pe", nc.tensor), ("act", nc.scalar), ("dve", nc.vector),
                          ("pool", nc.gpsimd), ("sp", nc.sync)):
            self.act[name] = dict(eng=eng, sem=nc.alloc_semaphore("s_" + name), cnt=0, seen={}, key=name)
        self.dsem = {}
        self.dval = {}
        self.dnext = {}
        for q in ("sp", "pool"):
            for i in range(ndma):
                self.dsem[(q, i)] = nc.alloc_semaphore("d_%s%d" % (q, i))
                self.dval[(q, i)] = 0
            self.dnext[q] = 0
        self.ndma = ndma

    def _sem(self, key):
        return self.act[key]["sem"] if key in self.act else self.dsem[key]

    def _wait(self, a, deps):
        need = {}
        for k, v in deps:
            if a["seen"].get(k, 0) >= v:
                continue
            need[k] = max(need.get(k, 0), v)
        for k, v in need.items():
            a["eng"].wait_ge(self._sem(k), v)
            a["seen"][k] = v

    def _deps(self, a, reads, writes, acc=False):
        d = []
        me = a["key"]
        for b in reads:
            for t in b.w:
                if not (me == "pe" and t[0] == "pe"):
                    d.append(t)
        for b in writes:
            if not acc:
                for t in b.w:
                    if not (me == "pe" and t[0] == "pe"):
                        d.append(t)
            for k, v in b.r.items():
                if k != me:
                    d.append((k, v))
        return d

    def _commit(self, tok, reads, writes, acc):
        for b in reads:
            b.r[tok[0]] = tok[1]
        for b in writes:
            if acc:
                b.w = [t for t in b.w if t[0] != tok[0]] + [tok]
            else:
                b.w = [tok]
                b.r = {}

    def op(self, actor, fn, reads=(), writes=()):
        a = self.act[actor]
        self._wait(a, self._deps(a, reads, writes))
        inst = fn(a["eng"])
        a["cnt"] += 1
        inst.then_inc(a["sem"], 1)
        self._commit((a["key"], a["cnt"]), reads, writes, False)

    def dma(self, actor, out, in_, reads=(), writes=(), acc=False):
        a = self.act[actor]
        self._wait(a, self._deps(a, reads, writes, acc))
        i = self.dnext[actor]
        self.dnext[actor] = (i + 1) % self.ndma
        key = (actor, i)
        if self.dval[key] > 0:
            self._wait(a, [(key, self.dval[key])])
        inst = a["eng"].dma_start(out=out, in_=in_)
        self.dval[key] += 16
        inst.then_inc(self.dsem[key], 16)
        self._commit((key, self.dval[key]), reads, writes, acc)

    def barrier(self):
        toks = [(k, a["cnt"]) for k, a in self.act.items() if a["cnt"] > 0]
        toks += [(k, v) for k, v in self.dval.items() if v > 0]
        for a in self.act.values():
            self._wait(a, [t for t in toks if t[0] != a["key"]])


def build(dbg=False, stop_after=99):
    nc = bass.Bass("TRN2", target_bir_lowering=False)

    def din(name, shape, dt=F32):
        return nc.dram_tensor(name, list(shape), dt, kind="ExternalInput").ap()

    def dscr(name, shape, dt):
        return nc.dram_tensor(name, list(shape), dt, kind=("ExternalOutput" if dbg else "Internal")).ap()

    xf = din("xf", [S, D])
    xo = din("xo", [OG, 528, D])
    mem = din("mem", [256, D])
    w_in = din("w_in", [D, 2048])
    w_pool = din("w_pool", [4, 128, 128])
    pscale = din("pscale", [128, 4])
    w_out = din("w_out", [D, D])
    g_mix = din("g_mix", [D]); g_cross = din("g_cross", [D]); g_mem = din("g_mem", [D])
    g_ffn = din("g_ffn", [D]); g_fin = din("g_fin", [D])
    wq = din("wq", [D, D]); wk = din("wk", [D, D]); wv = din("wv", [D, D]); wo = din("wo", [D, D])
    w_ge = din("w_ge", [D, 20])
    w_gate = din("w_gate", [NE, D, FF]); w_up = din("w_up", [NE, D, FF]); w_down = din("w_down", [NE, FF, D])
    ident = din("ident", [128, 128]); ui_d = din("ui", [128, 128]); ls_d = din("ls", [128, 128])
    masks_d = din("masks", [8, 128, 512])
    invcnt_d = din("invcnt", [128, 2048])
    y = nc.dram_tensor("y", [OG * 512, D], F32, kind="ExternalOutput").ap()

    qT_d = dscr("qT_d", [OG, 128, 2048], BF16)
    poT_d = dscr("poT_d", [OG, 128, 2048], BF16)
    aT_d = dscr("aT_d", [OG, 64, 4096], BF16)
    h1_d = dscr("h1_d", [OG * 512, D], F32)
    h2_d = dscr("h2_d", [OG * 512, D], F32)
    wg16 = nc.dram_tensor("wg16", [NE, D, FF], BF16, kind="Internal").ap()
    wu16 = nc.dram_tensor("wu16", [NE, D, FF], BF16, kind="Internal").ap()
    wd16 = nc.dram_tensor("wd16", [NE, FF, D], BF16, kind="Internal").ap()
    t_wg16 = [Tk() for _ in range(NE)]; t_wu16 = [Tk() for _ in range(NE)]; t_wd16 = [Tk() for _ in range(NE)]
    t_q = [Tk() for _ in range(OG)]; t_po = [Tk() for _ in range(OG)]; t_a = [Tk() for _ in range(OG)]
    t_h1 = [Tk() for _ in range(OG * 4)]; t_h2 = [Tk() for _ in range(OG * 4)]

    Sc = Sched(nc)
    op = Sc.op
    dma = Sc.dma

    root = ExitStack()
    with root:
        def sb(es, name, shape, dt):
            return es.enter_context(nc.sbuf_tensor(name, list(shape), dt))

        PS = [root.enter_context(nc.psum_tensor("ps%d" % i, [128, 512], F32)) for i in range(8)]
        t_PS = [Tk() for _ in range(8)]

        idb = sb(root, "idb", [128, 128], BF16); t_c = Tk()
        uib = sb(root, "uib", [128, 128], BF16)
        lsb = sb(root, "lsb", [128, 128], BF16)
        epsb = sb(root, "epsb", [128, 1], F32)
        junk = sb(root, "junk", [128, 1024], BF16)
        stats = [sb(root, "stat%d" % i, [128, 4], F32) for i in range(4)]
        t_stats = [Tk() for _ in range(4)]
        rot = {"st": 0, "x": 0, "xn": 0, "tp": 0, "mm": 0}
        t_eps = Tk()
        dma("pool", idb[:, :], ident[:, :], writes=[t_c])
        dma("pool", uib[:, :], ui_d[:, :], writes=[t_c], acc=True)
        dma("pool", lsb[:, :], ls_d[:, :], writes=[t_c], acc=True)
        op("dve", lambda e: e.memset(epsb[:, :], EPS), writes=[t_eps])

        def load_gain(tile, t, vec):
            dma("sp", tile[:, :], vec.partition_broadcast(128), writes=[t])

        def load_w3(tile, t, src, nchunk, c0, c1, p=128):
            w = c1 - c0
            for dc in range(nchunk):
                dma("pool", tile[0:p, dc * w:(dc + 1) * w], src[dc * p:(dc + 1) * p, c0:c1], writes=[t], acc=(dc > 0))

        def norm_block(xin, t_x, gain, t_g, xout, t_out, P=128):
            k = rot["st"]; rot["st"] = (k + 1) % 4
            st, tst = stats[k], t_stats[k]
            op("act", lambda e: e.activation(out=junk[0:P, :], in_=xin, func=AF.Square, accum_out=st[0:P, 0:1]),
               reads=[t_x], writes=[tst])
            op("act", lambda e: e.activation(out=st[0:P, 1:2], in_=st[0:P, 0:1], func=AF.Ln, scale=1.0 / D,
                                             bias=epsb[0:P, 0:1]), reads=[tst, t_eps], writes=[tst])
            op("act", lambda e: e.activation(out=st[0:P, 2:3], in_=st[0:P, 1:2], func=AF.Exp, scale=-0.5),
               reads=[tst], writes=[tst])
            op("dve", lambda e: e.scalar_tensor_tensor(out=xout, in0=xin, scalar=st[0:P, 2:3], in1=gain[0:P, :],
                                                       op0=ALU.mult, op1=ALU.mult),
               reads=[t_x, tst, t_g], writes=[t_out])

        def transpose_block(xn, t_xn, dst3, t_dst, P=128):
            k = rot["tp"]; rot["tp"] = (k + 1) % 2
            bank, tb = PS[k], t_PS[k]
            psb = bank[:, :].bitcast(BF16)
            for dc in range(8):
                op("pe", lambda e, dc=dc: e.transpose(out=psb[:, dc * P:(dc + 1) * P], in_=xn[0:P, dc * 128:(dc + 1) * 128],
                                                      identity=idb[0:P, 0:P]), reads=[t_xn, t_c], writes=[tb])
            op("act", lambda e: e.activation(out=dst3, in_=psb[:, 0:8 * P].rearrange("p (c t) -> p c t", c=8), func=AF.Copy),
               reads=[tb], writes=[t_dst])

        def norm_transpose_gen(blocks, xnb, t_xnb):
            prev = None
            for bk in list(blocks) + [None]:
                if bk is not None:
                    if bk.get("load"):
                        bk["load"]()
                    kn = rot["xn"]; rot["xn"] = (kn + 1) % 2
                    P = bk.get("P", 128)
                    norm_block(bk["xin"], bk["t_x"], bk["gain"], bk["t_g"], xnb[kn][0:P, :], t_xnb[kn], P=P)
                    bk["kn"] = kn
                if prev is not None:
                    transpose_block(xnb[prev["kn"]], t_xnb[prev["kn"]], prev["dst3"], prev["t_dst"], P=prev.get("P", 128))
                    if prev.get("post"):
                        prev["post"]()
                prev = bk
                yield

        def norm_transpose_seq(blocks, xnb, t_xnb):
            for _ in norm_transpose_gen(blocks, xnb, t_xnb):
                pass

        def drain(gen):
            if gen is not None:
                for _ in gen:
                    pass

        def mm_bank():
            k = rot["mm"]; rot["mm"] = (k + 1) % 4
            return PS[2 + k], t_PS[2 + k]

        def v3(tile, c):
            return tile[:, :].rearrange("p (c t) -> p c t", c=c)

        with ExitStack() as es:
            winPQ = sb(es, "winPQ", [128, 8 * 1024], BF16); t_winPQ = Tk()
            wpb = sb(es, "wpb", [128, 4 * 128], BF16); t_wpb = Tk()
            psc = sb(es, "psc", [128, 4], F32); t_psc = Tk()
            gmix = sb(es, "gmix", [128, D], F32); t_gmix = Tk()
            invc = sb(es, "invc", [128, 2048], F32); t_invc = Tk()
            xb = [sb(es, "xb%d" % i, [128, D], F32) for i in range(3)]; t_xb = [Tk() for _ in range(3)]
            xnb = [sb(es, "xnb%d" % i, [128, D], BF16) for i in range(2)]; t_xnb = [Tk() for _ in range(2)]
            xnT = [sb(es, "xnT%d" % i, [128, 8 * 528], BF16) for i in range(2)]; t_xnT = [Tk() for _ in range(2)]
            QTst = [sb(es, "QTst%d" % i, [128, 2048], BF16) for i in range(2)]; t_QTst = [Tk() for _ in range(2)]
            uTs = [sb(es, "uT%d" % i, [128, 4 * 528], F32) for i in range(2)]; t_uTs = [Tk() for _ in range(2)]
            tA = [sb(es, "tA%d" % i, [128, 528], F32) for i in range(4)]; t_tA = [Tk() for _ in range(4)]
            tB = [sb(es, "tB%d" % i, [128, 528], F32) for i in range(4)]; t_tB = [Tk() for _ in range(4)]
            pooleds = [sb(es, "pooled%d" % i, [128, 2048], BF16) for i in range(2)]
            t_pooleds = [[Tk() for _ in range(4)] for _ in range(2)]
            poSt = [sb(es, "poSt%d" % i, [128, 2048], BF16) for i in range(2)]; t_poSt = [Tk() for _ in range(2)]

            load_gain(gmix, t_gmix, g_mix)
            load_w3(winPQ, t_winPQ, w_in, 8, 0, 1024)
            for pg in range(4):
                dma("pool", wpb[:, pg * 128:(pg + 1) * 128], w_pool[pg, :, :], writes=[t_wpb], acc=(pg > 0))
            dma("sp", psc[:, :], pscale[:, :], writes=[t_psc])
            dma("sp", invc[:, :], invcnt_d[:, :], writes=[t_invc])

            def prep_1b(i):
                xT, t_xT = xnT[i % 2], t_xnT[i % 2]
                xT3 = v3(xT, 8)
                blocks = []
                for blk in range(-1, 4):
                    P = 16 if blk < 0 else 128
                    r0 = 0 if blk < 0 else 16 + blk * 128
                    kx = rot["x"]; rot["x"] = (kx + 1) % 3
                    blocks.append(dict(
                        load=(lambda kx=kx, P=P, r0=r0, i=i: dma("sp", xb[kx][0:P, :], xo[i, r0:r0 + P, :], writes=[t_xb[kx]])),
                        xin=xb[kx][0:P, :], t_x=t_xb[kx], gain=gmix, t_g=t_gmix, P=P,
                        dst3=xT3[:, :, r0:r0 + P], t_dst=t_xT))
                return norm_transpose_gen(blocks, xnb, t_xnb)

            def finish_pool(i):
                pooled, t_pooled = pooleds[i % 2], t_pooleds[i % 2]
                pst, t_pst = poSt[i % 2], t_poSt[i % 2]
                for pg in range(4):
                    bank, tb = mm_bank()
                    op("pe", lambda e, pg=pg, bank=bank: e.matmul(bank[:, :], lhsT=wpb[:, pg * 128:(pg + 1) * 128],
                                                                  rhs=pooled[:, pg * 512:(pg + 1) * 512], start=True, stop=True),
                       reads=[t_wpb, t_pooled[pg]], writes=[tb])
                    op("act", lambda e, pg=pg, bank=bank: e.activation(out=pst[:, pg * 512:(pg + 1) * 512], in_=bank[:, :],
                                                                       func=AF.Copy, scale=psc[:, pg:pg + 1]),
                       reads=[tb, t_psc], writes=[t_pst])
                dma("pool", poT_d[i, :, :], pst[:, :], reads=[t_pst], writes=[t_po[i]])

            pending = None
            drain(prep_1b(0))
            for i in range(OG):
                xT, t_xT = xnT[i % 2], t_xnT[i % 2]
                nxt = prep_1b(i + 1) if i + 1 < OG else None
                uT, t_uT = uTs[i % 2], t_uTs[i % 2]
                pooled, t_pooled = pooleds[i % 2], t_pooleds[i % 2]
                qs, t_qs = QTst[i % 2], t_QTst[i % 2]
                for c in range(4):
                    bank, tb = mm_bank()
                    for dc in range(8):
                        op("pe", lambda e, dc=dc, c=c, bank=bank: e.matmul(
                            bank[:, :], lhsT=winPQ[:, dc * 1024 + 512 + c * 128: dc * 1024 + 512 + (c + 1) * 128],
                            rhs=xT[:, dc * 528 + 16: dc * 528 + 528], start=(dc == 0), stop=(dc == 7)),
                           reads=[t_winPQ, t_xT], writes=[tb])
                    op("act", lambda e, c=c, bank=bank: e.activation(out=qs[:, c * 512:(c + 1) * 512], in_=bank[:, :],
                                                                     func=AF.Copy, scale=0.125), reads=[tb], writes=[t_qs])
                    if nxt is not None:
                        next(nxt, None)
                dma("pool", qT_d[i, :, :], qs[:, :], reads=[t_qs], writes=[t_q[i]])
                if pending is not None:
                    finish_pool(pending)
                    pending = None
                for pg in range(4):
                    bank, tb = mm_bank()
                    for dc in range(8):
                        op("pe", lambda e, dc=dc, pg=pg, bank=bank: e.matmul(
                            bank[:, :], lhsT=winPQ[:, dc * 1024 + pg * 128: dc * 1024 + (pg + 1) * 128],
                            rhs=xT[:, dc * 528 + 16: dc * 528 + 528], start=(dc == 0), stop=(dc == 7)),
                           reads=[t_winPQ, t_xT], writes=[tb])
                    op("act", lambda e, pg=pg, bank=bank: e.activation(out=uT[:, pg * 528 + 16: pg * 528 + 528], in_=bank[:, :],
                                                                       func=AF.Copy), reads=[tb], writes=[t_uT])
                    if nxt is not None:
                        next(nxt, None)
                bank, tb = mm_bank()
                for pg in range(4):
                    for dc in range(8):
                        op("pe", lambda e, dc=dc, pg=pg, bank=bank: e.matmul(
                            bank[:, pg * 16:(pg + 1) * 16], lhsT=winPQ[:, dc * 1024 + pg * 128: dc * 1024 + (pg + 1) * 128],
                            rhs=xT[:, dc * 528: dc * 528 + 16], start=(dc == 0), stop=(dc == 7)),
                           reads=[t_winPQ, t_xT], writes=[tb])
                op("act", lambda e, bank=bank: e.activation(out=v3(uT, 4)[:, :, 0:16],
                                                            in_=bank[:, 0:64].rearrange("p (c t) -> p c t", c=4),
                                                            func=AF.Copy), reads=[tb], writes=[t_uT])
                cur = [(uT, pg * 528, t_uT) for pg in range(4)]
                lo = [0, 0, 0, 0]
                for si, d in enumerate((1, 2, 4, 8)):
                    for pg in range(si, 4):
                        src, off, tsrc = cur[pg]
                        dst, tdst = (tA[pg], t_tA[pg]) if si % 2 == 0 else (tB[pg], t_tB[pg])
                        a = lo[pg] + d
                        op("dve", lambda e, src=src, off=off, dst=dst, a=a, d=d: e.tensor_tensor(
                            out=dst[:, a:528], in0=src[:, off + a: off + 528], in1=src[:, off + a - d: off + 528 - d],
                            op=ALU.add), reads=[tsrc], writes=[tdst])
                        cur[pg] = (dst, 0, tdst); lo[pg] = a
                for pg in range(4):
                    src, off, tsrc = cur[pg]
                    wdw = 2 ** (pg + 1)
                    dst, tdst = (tB[pg], t_tB[pg]) if src is tA[pg] else (tA[pg], t_tA[pg])
                    if i == 0:
                        op("dve", lambda e, src=src, dst=dst, pg=pg: e.tensor_tensor(
                            out=dst[:, 16:528], in0=src[:, 16:528], in1=invc[:, pg * 512:(pg + 1) * 512], op=ALU.mult),
                           reads=[tsrc, t_invc], writes=[tdst])
                    else:
                        op("dve", lambda e, src=src, dst=dst, wdw=wdw: e.tensor_scalar(
                            out=dst[:, 16:528], in0=src[:, 16:528], scalar1=1.0 / wdw, scalar2=None, op0=ALU.mult),
                           reads=[tsrc], writes=[tdst])
                    op("dve", lambda e, dst=dst, pg=pg: e.tensor_tensor(
                        out=pooled[:, pg * 512:(pg + 1) * 512], in0=dst[:, 16:528],
                        in1=uT[:, pg * 528 + 16: pg * 528 + 528], op=ALU.subtract),
                       reads=[tdst, t_uT], writes=[t_pooled[pg]])
                drain(nxt)
                pending = i
            finish_pool(pending)
            Sc.barrier()
        if stop_after <= 1:
            Sc.barrier()
            return nc

        with ExitStack() as eskv:
            KT = sb(eskv, "KT", [128, 4 * S], BF16)
            Vt = sb(eskv, "Vt", [128, 64 * 512], BF16)
            t_KT = [Tk() for _ in range(NG)]; t_V = [Tk() for _ in range(NG)]
            with ExitStack() as es:
                winKV = sb(es, "winKV", [128, 8 * 1024], BF16); t_winKV = Tk()
                gmix = sb(es, "gmix2", [128, D], F32); t_gmix = Tk()
                xb = [sb(es, "xc%d" % i, [128, D], F32) for i in range(3)]; t_xb = [Tk() for _ in range(3)]
                xnb = [sb(es, "xnc%d" % i, [128, D], BF16) for i in range(2)]; t_xnb = [Tk() for _ in range(2)]
                xnT = [sb(es, "xnTc%d" % i, [128, 8 * 512], BF16) for i in range(2)]; t_xnT = [Tk() for _ in range(2)]
                load_gain(gmix, t_gmix, g_mix)
                load_w3(winKV, t_winKV, w_in, 8, 1024, 2048)
                def prep_1a(g):
                    xT, t_xT = xnT[g % 2], t_xnT[g % 2]
                    xT3 = v3(xT, 8)
                    blocks = []
                    for blk in range(4):
                        r0 = g * 512 + blk * 128
                        kx = rot["x"]; rot["x"] = (kx + 1) % 3
                        blocks.append(dict(
                            load=(lambda kx=kx, r0=r0: dma("sp", xb[kx][:, :], xf[r0:r0 + 128, :], writes=[t_xb[kx]])),
                            xin=xb[kx][:, :], t_x=t_xb[kx], gain=gmix, t_g=t_gmix,
                            dst3=xT3[:, :, blk * 128:(blk + 1) * 128], t_dst=t_xT))
                    return norm_transpose_gen(blocks, xnb, t_xnb)

                drain(prep_1a(0))
                for g in range(NG):
                    xT, t_xT = xnT[g % 2], t_xnT[g % 2]
                    nxt = prep_1a(g + 1) if g + 1 < NG else None
                    for c in range(4):
                        bank, tb = mm_bank()
                        for dc in range(8):
                            op("pe", lambda e, dc=dc, c=c, bank=bank: e.matmul(
                                bank[:, :], lhsT=winKV[:, dc * 1024 + c * 128: dc * 1024 + (c + 1) * 128],
                                rhs=xT[:, dc * 512:(dc + 1) * 512], start=(dc == 0), stop=(dc == 7)),
                               reads=[t_winKV, t_xT], writes=[tb])
                        if c % 2 == 0:
                            op("act", lambda e, c=c, bank=bank, g=g: e.activation(
                                out=KT[:, c * S + g * 512: c * S + (g + 1) * 512], in_=bank[:, :], func=AF.Copy),
                               reads=[tb], writes=[t_KT[g]])
                        else:
                            op("dve", lambda e, c=c, bank=bank, g=g: e.tensor_copy(
                                out=KT[:, c * S + g * 512: c * S + (g + 1) * 512], in_=bank[:, :]),
                               reads=[tb], writes=[t_KT[g]])
                        if nxt is not None:
                            next(nxt, None)
                    for blk in range(4):
                        bank, tb = mm_bank()
                        for dc in range(8):
                            op("pe", lambda e, dc=dc, blk=blk, bank=bank: e.matmul(
                                bank[:, :], lhsT=xT[:, dc * 512 + blk * 128: dc * 512 + (blk + 1) * 128],
                                rhs=winKV[:, dc * 1024 + 512: dc * 1024 + 1024], start=(dc == 0), stop=(dc == 7)),
                               reads=[t_winKV, t_xT], writes=[tb])
                        kb = g * 4 + blk
                        op("dve", lambda e, kb=kb, bank=bank: e.tensor_copy(out=Vt[:, kb * 512:(kb + 1) * 512], in_=bank[:, :]),
                           reads=[tb], writes=[t_V[g]])
                        if nxt is not None and blk == 0:
                            next(nxt, None)
                    drain(nxt)
                Sc.barrier()

            if stop_after >= 3:
                with ExitStack() as es:
                    mk = sb(es, "mk", [128, 8 * 512], BF16); t_mk = Tk()
                    QTg = [sb(es, "QTg%d" % i, [128, 2048], BF16) for i in range(2)]; t_QTg = [Tk() for _ in range(2)]
                    NBUF = 4
                    e_sb = [sb(es, "e_sb%d" % i, [128, 512], F32) for i in range(NBUF)]; t_e = [Tk() for _ in range(NBUF)]
                    sp_sb = [sb(es, "sp_sb%d" % i, [128, 512], BF16) for i in range(NBUF)]; t_sp = [Tk() for _ in range(NBUF)]
                    G_sb = [sb(es, "G_sb%d" % i, [128, 512], F32) for i in range(NBUF)]; t_G = [Tk() for _ in range(NBUF)]
                    W_sb = [sb(es, "W_sb%d" % i, [128, 512], BF16) for i in range(NBUF)]; t_W = [Tk() for _ in range(NBUF)]
                    aTst = [sb(es, "aTst%d" % i, [64, 4096], BF16) for i in range(2)]; t_aTst = [Tk() for _ in range(2)]
                    for m in range(8):
                        dma("pool", mk[:, m * 512:(m + 1) * 512], masks_d[m, :, :], writes=[t_mk], acc=(m > 0))
                    for e_ in range(NE):
                        for hf in range(2):
                            dma("pool", wg16[e_, hf * 512:(hf + 1) * 512, :], w_gate[e_, hf * 512:(hf + 1) * 512, :],
                                writes=[t_wg16[e_]], acc=(hf > 0))
                            dma("pool", wu16[e_, hf * 512:(hf + 1) * 512, :], w_up[e_, hf * 512:(hf + 1) * 512, :],
                                writes=[t_wu16[e_]], acc=(hf > 0))
                            dma("pool", wd16[e_, hf * 256:(hf + 1) * 256, :], w_down[e_, hf * 256:(hf + 1) * 256, :],
                                writes=[t_wd16[e_]], acc=(hf > 0))
                    items = []
                    for i in range(OG):
                        E = 2 * i + 2
                        nkb = 4 * E
                        for hp in range(4):
                            for kb in range(nkb - 1, -1, -1):
                                for hh in range(2):
                                    m = kb - 4 * (E - 2)
                                    c0 = 128 * (m - 4) if m >= 4 else 0
                                    items.append(dict(i=i, hp=hp, kb=kb, hh=hh, first=(kb == nkb - 1), last=(kb == 0),
                                                      m=(m if m >= 0 else None), c0=c0))
                    NI = len(items)
                    zb = [PS[0], PS[1], PS[2], PS[3]]; t_zb = [t_PS[0], t_PS[1], t_PS[2], t_PS[3]]
                    Cb = [PS[4], PS[5]]; t_Cb = [t_PS[4], t_PS[5]]
                    Ob = [PS[6], PS[7]]; t_Ob = [t_PS[6], t_PS[7]]
                    loaded_q = set()

                    def ensure_q(i):
                        if i < OG and i not in loaded_q:
                            loaded_q.add(i)
                            dma("sp", QTg[i % 2][:, :], qT_d[i, :, :], reads=[t_q[i]], writes=[t_QTg[i % 2]])

                    def st_z(n):
                        it = items[n]; b = n % NBUF
                        ensure_q(it["i"])
                        if it["first"] and it["hp"] == 0 and it["hh"] == 0:
                            ensure_q(it["i"] + 1)
                        pl = it["hh"] * 64
                        g = it["kb"] // 4
                        q = QTg[it["i"] % 2]
                        msk = it["m"] is not None
                        c0 = it["c0"]
                        op("pe", lambda e: e.matmul(zb[b][:, c0:512], lhsT=KT[pl:pl + 64, it["hp"] * S + it["kb"] * 128: it["hp"] * S + (it["kb"] + 1) * 128],
                                                    rhs=q[pl:pl + 64, it["hp"] * 512 + c0:(it["hp"] + 1) * 512], start=True, stop=(not msk)),
                           reads=[t_KT[g], t_QTg[it["i"] % 2]], writes=[t_zb[b]])
                        if msk:
                            op("pe", lambda e: e.matmul(zb[b][:, c0:512], lhsT=idb[:, :], rhs=mk[:, it["m"] * 512 + c0:(it["m"] + 1) * 512],
                                                        start=False, stop=True), reads=[t_mk, t_c], writes=[t_zb[b]])

                    def st_e(n):
                        b = n % NBUF; c0 = items[n]["c0"]
                        op("act", lambda e: e.activation(out=e_sb[b][:, c0:512], in_=zb[b][:, c0:512], func=AF.Exp),
                           reads=[t_zb[b]], writes=[t_e[b]])

                    def st_sp(n):
                        b = n % NBUF; c0 = items[n]["c0"]
                        op("act", lambda e: e.activation(out=sp_sb[b][:, c0:512], in_=e_sb[b][:, c0:512], func=AF.Ln, bias=1.0),
                           reads=[t_e[b]], writes=[t_sp[b]])

                    def st_U(n):
                        it = items[n]; b = n % NBUF; h = it["hh"]
                        c0 = it["c0"]
                        op("pe", lambda e: e.matmul(Cb[h][:, c0:512], lhsT=uib[:, :], rhs=sp_sb[b][:, c0:512], start=it["first"], stop=True,
                                                    skip_group_check=True),
                           reads=[t_sp[b], t_c], writes=[t_Cb[h]])

                    def st_G(n):
                        it = items[n]; b = n % NBUF; h = it["hh"]
                        c0 = it["c0"]
                        op("act", lambda e: e.activation(out=G_sb[b][:, c0:512], in_=Cb[h][:, c0:512], func=AF.Exp, scale=-1.0),
                           reads=[t_Cb[h]], writes=[t_G[b]])

                    def st_L(n):
                        it = items[n]; b = n % NBUF; h = it["hh"]
                        c0 = it["c0"]
                        op("pe", lambda e: e.matmul(Cb[h][:, c0:512], lhsT=lsb[:, :], rhs=sp_sb[b][:, c0:512], start=False, stop=True,
                                                    skip_group_check=True),
                           reads=[t_sp[b], t_c], writes=[t_Cb[h]])

                    def st_W(n):
                        b = n % NBUF; c0 = items[n]["c0"]
                        op("dve", lambda e: e.tensor_tensor(out=W_sb[b][:, c0:512], in0=e_sb[b][:, c0:512], in1=G_sb[b][:, c0:512], op=ALU.mult),
                           reads=[t_e[b], t_G[b]], writes=[t_W[b]])

                    def st_PV(n):
                        it = items[n]; b = n % NBUF; h = it["hh"]
                        H = it["hp"] * 2 + h
                        g = it["kb"] // 4
                        c0 = it["c0"]
                        op("pe", lambda e: e.matmul(Ob[h][0:64, c0:512], lhsT=Vt[:, it["kb"] * 512 + H * 64: it["kb"] * 512 + (H + 1) * 64],
                                                    rhs=W_sb[b][:, c0:512], start=it["first"], stop=True, skip_group_check=True),
                           reads=[t_V[g], t_W[b]], writes=[t_Ob[h]])
                        if it["last"]:
                            ast, t_ast = aTst[it["i"] % 2], t_aTst[it["i"] % 2]
                            op("dve", lambda e: e.tensor_copy(out=ast[0:64, H * 512:(H + 1) * 512], in_=Ob[h][0:64, :]),
                               reads=[t_Ob[h]], writes=[t_ast])
                            if H == 7:
                                dma("sp", aT_d[it["i"], :, :], ast[:, :], reads=[t_ast], writes=[t_a[it["i"]]])

                    for n in range(-3, NI + 1):
                        if 0 <= n + 3 < NI:
                            st_z(n + 3)
                            st_e(n + 3)
                        if 0 <= n + 2 < NI:
                            st_sp(n + 2)
                        if 0 <= n + 1 < NI:
                            st_U(n + 1)
                            st_G(n + 1)
                        if 0 <= n < NI:
                            st_L(n)
                            st_W(n)
                        if 0 <= n - 1 < NI:
                            st_PV(n - 1)
                    Sc.barrier()
        if stop_after <= 3:
            Sc.barrier()
            return nc

        with ExitStack() as es:
            woP = sb(es, "woP", [128, 4 * 1024], BF16); t_woP = Tk()
            woA = sb(es, "woA", [128, 4 * 1024], BF16); t_woA = Tk()
            poT = [sb(es, "poT%d" % i, [128, 2048], BF16) for i in range(2)]; t_poT = [Tk() for _ in range(2)]
            aT = [sb(es, "aT%d" % i, [128, 2048], BF16) for i in range(2)]; t_aT = [Tk() for _ in range(2)]
            xb = [sb(es, "xd%d" % i, [128, D], F32) for i in range(3)]; t_xb = [Tk() for _ in range(3)]
            hst = [sb(es, "hst%d" % i, [128, D], F32) for i in range(3)]; t_hst = [Tk() for _ in range(3)]
            load_w3(woP, t_woP, w_out, 4, 0, 1024)
            load_w3(woA, t_woA, w_out[512:1024, :], 4, 0, 1024)
            kk = 0

            def load_mix(i):
                dma("sp", poT[i % 2][:, :], poT_d[i, :, :], reads=[t_po[i]], writes=[t_poT[i % 2]])
                for h in range(8):
                    dma("sp", aT[i % 2][(h % 2) * 64:(h % 2) * 64 + 64, (h // 2) * 512:(h // 2 + 1) * 512],
                        aT_d[i, :, h * 512:(h + 1) * 512], reads=[t_a[i]], writes=[t_aT[i % 2]], acc=(h > 0))

            load_mix(0)
            for i in range(OG):
                if i + 1 < OG:
                    load_mix(i + 1)
                p_, a_ = poT[i % 2], aT[i % 2]
                for blk in range(4):
                    kx = kk % 3; kk += 1
                    dma("sp", xb[kx][:, :], xo[i, 16 + blk * 128: 16 + (blk + 1) * 128, :], writes=[t_xb[kx]])
                    for half in range(2):
                        bank, tb = mm_bank()
                        for pg in range(4):
                            op("pe", lambda e, pg=pg, bank=bank, blk=blk, half=half: e.matmul(
                                bank[:, :], lhsT=p_[:, pg * 512 + blk * 128: pg * 512 + (blk + 1) * 128],
                                rhs=woP[:, pg * 1024 + half * 512: pg * 1024 + (half + 1) * 512], start=(pg == 0), stop=False),
                               reads=[t_woP, t_poT[i % 2]], writes=[tb])
                        for h in range(4):
                            op("pe", lambda e, h=h, bank=bank, blk=blk, half=half: e.matmul(
                                bank[:, :], lhsT=a_[:, h * 512 + blk * 128: h * 512 + (blk + 1) * 128],
                                rhs=woA[:, h * 1024 + half * 512: h * 1024 + (half + 1) * 512], start=False, stop=(h == 3)),
                               reads=[t_woA, t_aT[i % 2]], writes=[tb])
                        op("dve", lambda e, bank=bank, half=half, kx=kx: e.tensor_tensor(
                            out=hst[kx][:, half * 512:(half + 1) * 512], in0=bank[:, :], in1=xb[kx][:, half * 512:(half + 1) * 512],
                            op=ALU.add), reads=[tb, t_xb[kx]], writes=[t_hst[kx]])
                    dma("pool", h1_d[i * 512 + blk * 128: i * 512 + (blk + 1) * 128, :], hst[kx][:, :],
                        reads=[t_hst[kx]], writes=[t_h1[i * 4 + blk]])
            Sc.barrier()
        if stop_after <= 4:
            Sc.barrier()
            return nc

        with ExitStack() as es:
            KmT = sb(es, "KmT", [128, 8 * 256], BF16); t_KmT = Tk()
            Vm = sb(es, "Vm", [128, 2 * 1024], BF16); t_Vm = Tk()
            wqb = sb(es, "wqb", [128, 8 * 1024], BF16); t_wqb = Tk()
            wob = sb(es, "wob", [128, 8 * 1024], BF16); t_wob = Tk()
            gcr = sb(es, "gcr", [128, D], F32); t_gcr = Tk()
            load_gain(gcr, t_gcr, g_cross)
            load_w3(wqb, t_wqb, wq, 8, 0, 1024)
            load_w3(wob, t_wob, wo, 8, 0, 1024)
            xnb = [sb(es, "xne%d" % i, [128, D], BF16) for i in range(2)]; t_xnb = [Tk() for _ in range(2)]
            with ExitStack() as es2:
                wkb = sb(es2, "wkb", [128, 8 * 1024], BF16); t_wkb = Tk()
                wvb = sb(es2, "wvb", [128, 8 * 1024], BF16); t_wvb = Tk()
                gme = sb(es2, "gme", [128, D], F32); t_gme = Tk()
                mb_ = [sb(es2, "memb%d" % i, [128, D], F32) for i in range(2)]; t_mb = [Tk() for _ in range(2)]
                memT = sb(es2, "memT", [128, 8 * 256], BF16); t_memT = Tk()
                load_gain(gme, t_gme, g_mem)
                load_w3(wkb, t_wkb, wk, 8, 0, 1024)
                load_w3(wvb, t_wvb, wv, 8, 0, 1024)
                for k in range(2):
                    dma("sp", mb_[k][:, :], mem[k * 128:(k + 1) * 128, :], writes=[t_mb[k]])
                    norm_block(mb_[k][:, :], t_mb[k], gme, t_gme, xnb[k][:, :], t_xnb[k])
                    transpose_block(xnb[k], t_xnb[k], v3(memT, 8)[:, :, k * 128:(k + 1) * 128], t_memT)
                for ec in range(8):
                    bank, tb = mm_bank()
                    for dc in range(8):
                        op("pe", lambda e, dc=dc, ec=ec, bank=bank: e.matmul(
                            bank[:, 0:256], lhsT=wkb[:, dc * 1024 + ec * 128: dc * 1024 + (ec + 1) * 128],
                            rhs=memT[:, dc * 256:(dc + 1) * 256], start=(dc == 0), stop=(dc == 7)),
                           reads=[t_wkb, t_memT], writes=[tb])
                    op("act", lambda e, ec=ec, bank=bank: e.activation(out=KmT[:, ec * 256:(ec + 1) * 256], in_=bank[:, 0:256], func=AF.Copy),
                       reads=[tb], writes=[t_KmT])
                for k in range(2):
                    for half in range(2):
                        bank, tb = mm_bank()
                        for dc in range(8):
                            op("pe", lambda e, dc=dc, k=k, half=half, bank=bank: e.matmul(
                                bank[:, :], lhsT=memT[:, dc * 256 + k * 128: dc * 256 + (k + 1) * 128],
                                rhs=wvb[:, dc * 1024 + half * 512: dc * 1024 + (half + 1) * 512], start=(dc == 0), stop=(dc == 7)),
                               reads=[t_wvb, t_memT], writes=[tb])
                        op("act", lambda e, k=k, half=half, bank=bank: e.activation(
                            out=Vm[:, k * 1024 + half * 512: k * 1024 + (half + 1) * 512], in_=bank[:, :], func=AF.Copy),
                           reads=[tb], writes=[t_Vm])
                Sc.barrier()
            h1g = [sb(es, "h1g%d" % i, [128, 4 * D], F32) for i in range(2)]; t_h1g = [[Tk() for _ in range(4)] for _ in range(2)]
            xcT = [sb(es, "xcT%d" % i, [128, 8 * 512], BF16) for i in range(2)]; t_xcT = [Tk() for _ in range(2)]
            qmT = sb(es, "qmT", [128, 8 * 512], BF16); t_qmT = Tk()
            pE = [sb(es, "pE%d" % i, [128, 256], F32) for i in range(4)]; t_pE = [Tk() for _ in range(4)]
            pn = [sb(es, "pn%d" % i, [128, 256], BF16) for i in range(4)]; t_pn = [Tk() for _ in range(4)]
            sm = [sb(es, "sm%d" % i, [128, 4], F32) for i in range(8)]; t_sm = [Tk() for _ in range(8)]
            pTa = sb(es, "pTa", [128, 8 * 512], BF16); t_pTa = Tk()
            oT = sb(es, "oT", [128, 8 * 512], BF16); t_oT = Tk()
            hst = [sb(es, "hse%d" % i, [128, D], F32) for i in range(2)]; t_hst = [Tk() for _ in range(2)]
            ks = 0; kh = 0

            def prep_3c(i):
                hg, thg = h1g[i % 2], t_h1g[i % 2]
                xc3 = v3(xcT[i % 2], 8)
                blocks = []
                for blk in range(4):
                    blocks.append(dict(
                        load=(lambda blk=blk, hg=hg, thg=thg, i=i: dma(
                            "sp", hg[:, blk * D:(blk + 1) * D], h1_d[i * 512 + blk * 128: i * 512 + (blk + 1) * 128, :],
                            reads=[t_h1[i * 4 + blk]], writes=[thg[blk]])),
                        xin=hg[:, blk * D:(blk + 1) * D], t_x=thg[blk], gain=gcr, t_g=t_gcr,
                        dst3=xc3[:, :, blk * 128:(blk + 1) * 128], t_dst=t_xcT[i % 2]))
                return norm_transpose_gen(blocks, xnb, t_xnb)

            drain(prep_3c(0))
            for i in range(OG):
                hg, thg = h1g[i % 2], t_h1g[i % 2]
                xc, t_xc = xcT[i % 2], t_xcT[i % 2]
                nxt = prep_3c(i + 1) if i + 1 < OG else None
                for ec in range(8):
                    bank, tb = mm_bank()
                    for dc in range(8):
                        op("pe", lambda e, dc=dc, ec=ec, bank=bank: e.matmul(
                            bank[:, :], lhsT=wqb[:, dc * 1024 + ec * 128: dc * 1024 + (ec + 1) * 128],
                            rhs=xc[:, dc * 512:(dc + 1) * 512], start=(dc == 0), stop=(dc == 7)),
                           reads=[t_wqb, t_xc], writes=[tb])
                    op("act", lambda e, ec=ec, bank=bank: e.activation(out=qmT[:, ec * 512:(ec + 1) * 512], in_=bank[:, :],
                                                                       func=AF.Copy, scale=1.0 / 16.0), reads=[tb], writes=[t_qmT])
                for blk in range(4):
                    banks = [mm_bank() for _ in range(4)]
                    smt = []
                    for hh in range(4):
                        bank, tb = banks[hh]
                        for c2 in range(2):
                            ec = hh * 2 + c2
                            op("pe", lambda e, ec=ec, c2=c2, blk=blk, bank=bank: e.matmul(
                                bank[:, 0:256], lhsT=qmT[:, ec * 512 + blk * 128: ec * 512 + (blk + 1) * 128],
                                rhs=KmT[:, ec * 256:(ec + 1) * 256], start=(c2 == 0), stop=(c2 == 1)),
                               reads=[t_qmT, t_KmT], writes=[tb])
                        smt.append((sm[ks % 8], t_sm[ks % 8])); ks += 1
                    for hh in range(4):
                        bank, tb = banks[hh]; s_, ts_ = smt[hh]
                        op("dve", lambda e, bank=bank, s_=s_: e.tensor_reduce(out=s_[:, 0:1], in_=bank[:, 0:256], axis=AX.X, op=ALU.max),
                           reads=[tb], writes=[ts_])
                    for hh in range(4):
                        s_, ts_ = smt[hh]
                        op("dve", lambda e, s_=s_: e.tensor_scalar(out=s_[:, 1:2], in0=s_[:, 0:1], scalar1=-1.0, scalar2=None, op0=ALU.mult),
                           reads=[ts_], writes=[ts_])
                    for hh in range(4):
                        bank, tb = banks[hh]; s_, ts_ = smt[hh]
                        op("act", lambda e, bank=bank, s_=s_, hh=hh: e.activation(out=pE[hh][:, :], in_=bank[:, 0:256], func=AF.Exp,
                                                                                 bias=s_[:, 1:2], accum_out=s_[:, 2:3]),
                           reads=[tb, ts_], writes=[t_pE[hh], ts_])
                    for hh in range(4):
                        s_, ts_ = smt[hh]
                        op("dve", lambda e, s_=s_: e.reciprocal(out=s_[:, 3:4], in_=s_[:, 2:3]), reads=[ts_], writes=[ts_])
                    for hh in range(4):
                        s_, ts_ = smt[hh]
                        op("dve", lambda e, s_=s_, hh=hh: e.tensor_scalar(out=pn[hh][:, :], in0=pE[hh][:, :], scalar1=s_[:, 3:4],
                                                                         scalar2=None, op0=ALU.mult),
                           reads=[t_pE[hh], ts_], writes=[t_pn[hh]])
                    k = rot["tp"]; rot["tp"] = (k + 1) % 2
                    tbank, ttb = PS[k], t_PS[k]
                    psb = tbank[:, :].bitcast(BF16)
                    for hh in range(4):
                        for mb2 in range(2):
                            j = hh * 2 + mb2
                            op("pe", lambda e, mb2=mb2, hh=hh, j=j, psb=psb: e.transpose(
                                out=psb[:, j * 128:(j + 1) * 128], in_=pn[hh][:, mb2 * 128:(mb2 + 1) * 128], identity=idb[:, :]),
                               reads=[t_pn[hh], t_c], writes=[ttb])
                    op("act", lambda e, psb=psb, blk=blk: e.activation(
                        out=v3(pTa, 8)[:, :, blk * 128:(blk + 1) * 128], in_=psb[:, 0:1024].rearrange("p (c t) -> p c t", c=8),
                        func=AF.Copy), reads=[ttb], writes=[t_pTa])
                    if nxt is not None:
                        next(nxt, None)
                for ec in range(8):
                    hh = ec // 2
                    bank, tb = mm_bank()
                    for mb2 in range(2):
                        op("pe", lambda e, mb2=mb2, ec=ec, hh=hh, bank=bank: e.matmul(
                            bank[:, :], lhsT=Vm[:, mb2 * 1024 + ec * 128: mb2 * 1024 + (ec + 1) * 128],
                            rhs=pTa[:, (hh * 2 + mb2) * 512:(hh * 2 + mb2 + 1) * 512], start=(mb2 == 0), stop=(mb2 == 1)),
                           reads=[t_Vm, t_pTa], writes=[tb])
                    op("act", lambda e, ec=ec, bank=bank: e.activation(out=oT[:, ec * 512:(ec + 1) * 512], in_=bank[:, :], func=AF.Copy),
                       reads=[tb], writes=[t_oT])
                    if nxt is not None and ec == 0:
                        next(nxt, None)
                for blk in range(4):
                    kx = kh % 2; kh += 1
                    for half in range(2):
                        bank, tb = mm_bank()
                        for ec in range(8):
                            op("pe", lambda e, ec=ec, bank=bank, blk=blk, half=half: e.matmul(
                                bank[:, :], lhsT=oT[:, ec * 512 + blk * 128: ec * 512 + (blk + 1) * 128],
                                rhs=wob[:, ec * 1024 + half * 512: ec * 1024 + (half + 1) * 512], start=(ec == 0), stop=(ec == 7)),
                               reads=[t_wob, t_oT], writes=[tb])
                        op("dve", lambda e, bank=bank, half=half, kx=kx, blk=blk, hg=hg: e.tensor_tensor(
                            out=hst[kx][:, half * 512:(half + 1) * 512], in0=bank[:, :],
                            in1=hg[:, blk * D + half * 512: blk * D + (half + 1) * 512], op=ALU.add),
                           reads=[tb, thg[blk]], writes=[t_hst[kx]])
                    dma("pool", h2_d[i * 512 + blk * 128: i * 512 + (blk + 1) * 128, :], hst[kx][:, :],
                        reads=[t_hst[kx]], writes=[t_h2[i * 4 + blk]])
                drain(nxt)
            Sc.barrier()
        if stop_after <= 5:
            Sc.barrier()
            return nc

        with ExitStack() as es:
            TB = 8
            NT = OG * 4 // TB
            TW = TB * 128
            hacc = [sb(es, "hacc%d" % i, [128, TB * D], F32) for i in range(2)]
            t_hacc = [[Tk() for _ in range(TB)] for _ in range(2)]
            xfT = [sb(es, "xfT%d" % i, [128, 8 * TW], BF16) for i in range(2)]; t_xfT = [Tk() for _ in range(2)]
            comb = [sb(es, "comb%d" % i, [128, TB * 16], F32) for i in range(2)]
            t_comb = [[Tk() for _ in range(TB)] for _ in range(2)]
            rt = [sb(es, "rt%d" % i, [128, 64], F32) for i in range(16)]; t_rt = [Tk() for _ in range(16)]
            xnb = [sb(es, "xnf%d" % i, [128, D], BF16) for i in range(2)]; t_xnb = [Tk() for _ in range(2)]
            gff = sb(es, "gff", [128, D], F32); t_gff = Tk()
            gfn = sb(es, "gfn", [128, D], F32); t_gfn = Tk()
            wgeb = sb(es, "wgeb", [128, 8 * 20], BF16); t_wgeb = Tk()
            wgb = [sb(es, "wgb%d" % i, [128, 8 * FF], BF16) for i in range(2)]; t_wgb = [Tk() for _ in range(2)]
            wub = [sb(es, "wub%d" % i, [128, 8 * FF], BF16) for i in range(2)]; t_wub = [Tk() for _ in range(2)]
            wdb = [sb(es, "wdb%d" % i, [128, 4 * D], BF16) for i in range(1)]; t_wdb = [Tk() for _ in range(1)]
            hmT = [sb(es, "hmT%d" % i, [128, 4 * TW], BF16) for i in range(1)]; t_hmT = [Tk() for _ in range(1)]
            sg = [sb(es, "sg%d" % i, [128, 512], F32) for i in range(2)]; t_sg = [Tk() for _ in range(2)]
            ost = [sb(es, "ost%d" % i, [128, D], F32) for i in range(2)]; t_ost = [Tk() for _ in range(2)]
            load_gain(gff, t_gff, g_ffn)
            load_gain(gfn, t_gfn, g_fin)
            load_w3(wgeb, t_wgeb, w_ge, 8, 0, 20)
            cnt = {"sg": 0, "ost": 0}

            def load_gu(e_):
                p = e_ % 2
                for dc in range(8):
                    dma("pool", wgb[p][:, dc * FF:(dc + 1) * FF], wg16[e_, dc * 128:(dc + 1) * 128, :], reads=[t_wg16[e_]], writes=[t_wgb[p]], acc=(dc > 0))
                for dc in range(8):
                    dma("pool", wub[p][:, dc * FF:(dc + 1) * FF], wu16[e_, dc * 128:(dc + 1) * 128, :], reads=[t_wu16[e_]], writes=[t_wub[p]], acc=(dc > 0))

            def load_d(e_):
                for fc in range(4):
                    dma("pool", wdb[0][:, fc * D:(fc + 1) * D], wd16[e_, fc * 128:(fc + 1) * 128, :], reads=[t_wd16[e_]], writes=[t_wdb[0]], acc=(fc > 0))

            def prologue_gen(ti):
                bs = ti % 2
                hac, thac, xT, t_xT, cmb, t_cmb = hacc[bs], t_hacc[bs], xfT[bs], t_xfT[bs], comb[bs], t_comb[bs]
                blocks = []
                for blk in range(TB):
                    gb = ti * TB + blk
                    hb = hac[:, blk * D:(blk + 1) * D]
                    blocks.append(dict(
                        load=(lambda hb=hb, gb=gb, blk=blk: dma("sp", hb, h2_d[gb * 128:(gb + 1) * 128, :],
                                                                 reads=[t_h2[gb]], writes=[thac[blk]])),
                        xin=hb, t_x=thac[blk], gain=gff, t_g=t_gff,
                        dst3=v3(xT, 8)[:, :, blk * 128:(blk + 1) * 128], t_dst=t_xT))
                yield from norm_transpose_gen(blocks, xnb, t_xnb)
                RB = []
                for blk in range(TB):
                    bank, tb = mm_bank()
                    for dc in range(8):
                        op("pe", lambda e, dc=dc, bank=bank, blk=blk: e.matmul(
                            bank[:, 0:20], lhsT=xT[:, dc * TW + blk * 128: dc * TW + (blk + 1) * 128],
                            rhs=wgeb[:, dc * 20:(dc + 1) * 20], start=(dc == 0), stop=(dc == 7)),
                           reads=[t_xT, t_wgeb], writes=[tb])
                    R, tR = rt[bs * 8 + blk], t_rt[bs * 8 + blk]
                    op("dve", lambda e, bank=bank, R=R: e.tensor_copy(out=R[:, 0:20], in_=bank[:, 0:20]), reads=[tb], writes=[tR])
                    RB.append((R, tR))
                    yield
                stages = [
                    ("dve", lambda e, R, blk: e.tensor_reduce(out=R[:, 20:21], in_=R[:, 0:4], axis=AX.X, op=ALU.max)),
                    ("dve", lambda e, R, blk: e.tensor_scalar(out=R[:, 21:22], in0=R[:, 20:21], scalar1=-1.0, scalar2=None, op0=ALU.mult)),
                    ("act", lambda e, R, blk: e.activation(out=R[:, 57:61], in_=R[:, 0:4], func=AF.Exp, bias=R[:, 21:22], accum_out=R[:, 22:23])),
                    ("dve", lambda e, R, blk: e.reciprocal(out=R[:, 23:24], in_=R[:, 22:23])),
                    ("dve", lambda e, R, blk: e.tensor_scalar(out=R[:, 24:28], in0=R[:, 0:4], scalar1=R[:, 20:21], scalar2=None, op0=ALU.is_ge)),
                    ("dve", lambda e, R, blk: e.tensor_scalar(out=R[:, 28:32], in0=R[:, 4:8], scalar1=R[:, 24:25], scalar2=None, op0=ALU.mult)),
                ]
                for g_ in range(1, 4):
                    stages.append(("dve", lambda e, R, blk, g_=g_: e.scalar_tensor_tensor(
                        out=R[:, 28:32], in0=R[:, 4 + 4 * g_: 8 + 4 * g_], scalar=R[:, 24 + g_: 25 + g_], in1=R[:, 28:32], op0=ALU.mult, op1=ALU.add)))
                stages += [
                    ("dve", lambda e, R, blk: e.tensor_reduce(out=R[:, 32:33], in_=R[:, 28:32], axis=AX.X, op=ALU.max)),
                    ("dve", lambda e, R, blk: e.tensor_scalar(out=R[:, 33:37], in0=R[:, 28:32], scalar1=R[:, 32:33], scalar2=None, op0=ALU.is_ge)),
                    ("dve", lambda e, R, blk: e.scalar_tensor_tensor(out=R[:, 37:41], in0=R[:, 33:37], scalar=-1e30, in1=R[:, 28:32], op0=ALU.mult, op1=ALU.add)),
                    ("dve", lambda e, R, blk: e.tensor_reduce(out=R[:, 41:42], in_=R[:, 37:41], axis=AX.X, op=ALU.max)),
                    ("dve", lambda e, R, blk: e.tensor_scalar(out=R[:, 42:46], in0=R[:, 37:41], scalar1=R[:, 41:42], scalar2=None, op0=ALU.is_ge)),
                    ("dve", lambda e, R, blk: e.tensor_tensor(out=R[:, 46:47], in0=R[:, 41:42], in1=R[:, 32:33], op=ALU.subtract)),
                    ("act", lambda e, R, blk: e.activation(out=R[:, 47:48], in_=R[:, 46:47], func=AF.Exp)),
                    ("dve", lambda e, R, blk: e.tensor_scalar(out=R[:, 47:48], in0=R[:, 47:48], scalar1=1.0, scalar2=None, op0=ALU.add)),
                    ("dve", lambda e, R, blk: e.reciprocal(out=R[:, 47:48], in_=R[:, 47:48])),
                    ("dve", lambda e, R, blk: e.tensor_scalar(out=R[:, 48:49], in0=R[:, 47:48], scalar1=-1.0, scalar2=1.0, op0=ALU.mult, op1=ALU.add)),
                    ("dve", lambda e, R, blk: e.tensor_scalar(out=R[:, 49:53], in0=R[:, 33:37], scalar1=R[:, 47:48], scalar2=None, op0=ALU.mult)),
                    ("dve", lambda e, R, blk: e.scalar_tensor_tensor(out=R[:, 49:53], in0=R[:, 42:46], scalar=R[:, 48:49], in1=R[:, 49:53], op0=ALU.mult, op1=ALU.add)),
                    ("dve", lambda e, R, blk: e.tensor_scalar(out=R[:, 53:57], in0=R[:, 24:28], scalar1=R[:, 23:24], scalar2=None, op0=ALU.mult)),
                ]
                for eng, fn in stages:
                    for blk in range(TB):
                        R, tR = RB[blk]
                        op(eng, (lambda e, R=R, blk=blk, fn=fn: fn(e, R, blk)), reads=[tR], writes=[tR])
                    yield
                for g_ in range(4):
                    for blk in range(TB):
                        R, tR = RB[blk]
                        op("dve", lambda e, g_=g_, blk=blk, R=R: e.tensor_scalar(out=cmb[:, blk * 16 + g_ * 4: blk * 16 + g_ * 4 + 4], in0=R[:, 49:53],
                                                                                 scalar1=R[:, 53 + g_: 54 + g_], scalar2=None, op0=ALU.mult),
                           reads=[tR], writes=[t_cmb[blk]])
                    yield

            def epilogue_gen(ti):
                bs = ti % 2
                for blk in range(TB):
                    gb = ti * TB + blk
                    ko = cnt["ost"] % 2; cnt["ost"] += 1
                    hb = hacc[bs][:, blk * D:(blk + 1) * D]
                    norm_block(hb, t_hacc[bs][blk], gfn, t_gfn, ost[ko][:, :], t_ost[ko])
                    dma("pool", y[gb * 128:(gb + 1) * 128, :], ost[ko][:, :], reads=[t_ost[ko]], writes=[Tk()])
                    yield

            def chain(*gens):
                for g_ in gens:
                    if g_ is not None:
                        yield from g_

            def expert(ti, e_, tick):
                bs = ti % 2
                p = e_ % 2
                xT, t_xT = xfT[bs], t_xfT[bs]
                hm, thm = hmT[0], t_hmT[0]
                for th in range(TB // 4):
                    for fc in range(4):
                        bg, tbg = mm_bank()
                        bu, tbu = mm_bank()
                        for dc in range(8):
                            op("pe", lambda e, dc=dc, fc=fc, th=th, bg=bg: e.matmul(
                                bg[:, :], lhsT=wgb[p][:, dc * FF + fc * 128: dc * FF + (fc + 1) * 128],
                                rhs=xT[:, dc * TW + th * 512: dc * TW + (th + 1) * 512], start=(dc == 0), stop=(dc == 7)),
                               reads=[t_wgb[p], t_xT], writes=[tbg])
                        for dc in range(8):
                            op("pe", lambda e, dc=dc, fc=fc, th=th, bu=bu: e.matmul(
                                bu[:, :], lhsT=wub[p][:, dc * FF + fc * 128: dc * FF + (fc + 1) * 128],
                                rhs=xT[:, dc * TW + th * 512: dc * TW + (th + 1) * 512], start=(dc == 0), stop=(dc == 7)),
                               reads=[t_wub[p], t_xT], writes=[tbu])
                        s_, ts_ = sg[cnt["sg"] % 2], t_sg[cnt["sg"] % 2]; cnt["sg"] += 1
                        op("act", lambda e, bg=bg, s_=s_: e.activation(out=s_[:, :], in_=bg[:, :], func=AF.Silu),
                           reads=[tbg], writes=[ts_])
                        op("dve", lambda e, bu=bu, s_=s_, fc=fc, th=th, hm=hm: e.tensor_tensor(
                            out=hm[:, fc * TW + th * 512: fc * TW + (th + 1) * 512], in0=bu[:, :], in1=s_[:, :], op=ALU.mult),
                           reads=[tbu, ts_], writes=[thm])
                        tick()
                for blk in range(TB):
                    for half in range(2):
                        bank, tb = mm_bank()
                        for fc in range(4):
                            op("pe", lambda e, fc=fc, blk=blk, half=half, bank=bank, hm=hm: e.matmul(
                                bank[:, :], lhsT=hm[:, fc * TW + blk * 128: fc * TW + (blk + 1) * 128],
                                rhs=wdb[0][:, fc * D + half * 512: fc * D + (half + 1) * 512], start=(fc == 0), stop=(fc == 3)),
                               reads=[t_wdb[0], thm], writes=[tb])
                        hb = hacc[bs][:, blk * D + half * 512: blk * D + (half + 1) * 512]
                        op("dve", lambda e, bank=bank, hb=hb, blk=blk, e_=e_: e.scalar_tensor_tensor(
                            out=hb, in0=bank[:, :], scalar=comb[bs][:, blk * 16 + e_: blk * 16 + e_ + 1], in1=hb, op0=ALU.mult, op1=ALU.add),
                           reads=[tb, t_comb[bs][blk], t_hacc[bs][blk]], writes=[t_hacc[bs][blk]])
                        tick()

            load_gu(0)
            load_d(0)
            drain(prologue_gen(0))
            for ti in range(NT):
                side = chain(epilogue_gen(ti - 1) if ti > 0 else None,
                             prologue_gen(ti + 1) if ti + 1 < NT else None)
                state = {"n": 0}

                def tick(side=side, state=state):
                    state["n"] += 1
                    if state["n"] % 5 == 0:
                        next(side, None)

                for e_ in range(NE):
                    nxt_e = (e_ + 1) % NE
                    if not (ti == NT - 1 and e_ == NE - 1):
                        load_gu(nxt_e)
                    expert(ti, e_, tick)
                    if not (ti == NT - 1 and e_ == NE - 1):
                        load_d(nxt_e)
                drain(side)
            drain(epilogue_gen(NT - 1))
            Sc.barrier()
        Sc.barrier()
    return nc


_NC_CACHE = {}


def _consts(r):
    s = np.arange(128)[:, None]
    t = np.arange(512)[None, :]
    Dm = [((128 * jj + s) < t).astype(np.float32) for jj in range(4)]
    Z = np.zeros((128, 512), np.float32)
    O = np.ones((128, 512), np.float32)
    masks = np.stack(Dm + [Z] * 4) if r == 0 else np.stack([O] * 4 + Dm)
    masks = (masks - 1.0) * 30000.0
    inv = np.zeros((128, 4, 512), np.float32)
    for pg in range(4):
        w = 2 ** (pg + 1)
        if r == 0:
            inv[:, pg, :] = 1.0 / np.minimum(np.arange(512) + 1, w)[None, :]
        else:
            inv[:, pg, :] = 1.0 / w
    j = np.arange(128)[:, None]
    c = np.arange(128)[None, :]
    ui = (j >= c).astype(np.float32)
    ls = (j < c).astype(np.float32)
    return masks, inv.reshape(128, 2048), ui, ls


def make_in_maps(inp):
    f = lambda a: np.ascontiguousarray(np.asarray(a, dtype=np.float32))
    x = f(inp["x"]); memv = f(inp["mem"])
    shared = dict(
        w_in=f(inp["w_in"][0]), w_pool=f(inp["w_pool"][0]),
        pscale=f(np.asarray(inp["pool_scale"][0]).reshape(4, 128).T),
        w_out=f(inp["w_out"][0]), g_mix=f(inp["norm_mix"][0]), g_cross=f(inp["norm_cross"][0]),
        g_mem=f(inp["norm_mem"][0]), g_ffn=f(inp["norm_ffn"][0]), g_fin=f(inp["norm_final"]),
        wq=f(inp["w_q_mem"][0]), wk=f(inp["w_k_mem"][0]), wv=f(inp["w_v_mem"][0]), wo=f(inp["w_o_mem"][0]),
        w_ge=f(np.concatenate([np.asarray(inp["w_group"][0]), np.asarray(inp["w_expert"][0])], axis=1)),
        w_gate=f(inp["w_gate"][0]), w_up=f(inp["w_up"][0]), w_down=f(inp["w_down"][0]),
        ident=np.eye(128, dtype=np.float32),
    )
    maps = []
    for c in range(8):
        b, r = c // 2, c % 2
        masks, inv, ui, ls = _consts(r)
        xo = np.zeros((OG, 528, D), np.float32)
        for i in range(OG):
            g = 2 * i + r
            s0 = g * 512
            if s0 >= 16:
                xo[i] = x[b, s0 - 16:s0 + 512]
            else:
                xo[i, 16:] = x[b, 0:512]
        m = dict(shared)
        m.update(xf=x[b], xo=xo, mem=memv[b], masks=masks, invcnt=inv, ui=ui, ls=ls)
        maps.append(m)
    return maps


def kernel(**inputs):
    if "nc" not in _NC_CACHE:
        _NC_CACHE["nc"] = build()
    nc = _NC_CACHE["nc"]
    maps = make_in_maps(inputs)
    res = run_bass_kernel_spmd(nc, maps, core_ids=list(range(8)))
    out = np.zeros((4, S, D), np.float32)
    for c in range(8):
        b, r = c // 2, c % 2
        yc = np.asarray(res.results[c]["y"]).reshape(OG, 512, D)
        for i in range(OG):
            g = 2 * i + r
            out[b, g * 512:(g + 1) * 512] = yc[i]
    return out
```
